# Optimizing a Trainium2 kernel written in Bass

```python
import jax
import jax.numpy as jnp
from jax import lax
import numpy as np

D_MODEL = 2048
BATCH = 16
SEQ = 2048
DEPTH = 2

GRID_W = 64
CTX_LEN = 256
F32 = jnp.float32

ATT_HEAD_DIM = 128
ATT_HEADS = D_MODEL // 256
ATT_KV_HEADS = ATT_HEADS // 4
ATT_W = ATT_HEADS * ATT_HEAD_DIM
ATT_KV_W = ATT_KV_HEADS * ATT_HEAD_DIM
ATT_SCALE = ATT_HEAD_DIM ** -0.5
ROPE_THETA = 10000.0
ROPE_FREQS = ATT_HEAD_DIM // 4
Q_BLOCK = 128

RWKV_HEAD_DIM = 64
RWKV_W = D_MODEL // 2
RWKV_HEADS = RWKV_W // RWKV_HEAD_DIM
DECAY_RANK = 64
ICLR_RANK = 64
GATE_RANK = 128
RWKV_SPLITS = (RWKV_W, RWKV_W, RWKV_W, 2 * DECAY_RANK, 2 * ICLR_RANK, GATE_RANK)
RWKV_IN_W = sum(RWKV_SPLITS)
RWKV_OFFSETS = tuple(int(o) for o in np.cumsum(RWKV_SPLITS)[:-1])
GN_EPS = 64e-5

CONV_W = D_MODEL // 2
CONV_K = 31

N_BRANCH = 3
IN_SPLITS = (ATT_W, ATT_KV_W, ATT_KV_W, RWKV_IN_W, 2 * CONV_W, N_BRANCH * D_MODEL)
IN_W = sum(IN_SPLITS)
IN_OFFSETS = tuple(int(o) for o in np.cumsum(IN_SPLITS)[:-1])

N_GROUPS = 4
EXPERTS_PER_GROUP = 8
N_EXPERTS = N_GROUPS * EXPERTS_PER_GROUP
TOP_K = 2
EXPERT_FF = D_MODEL // 2
MOE_BLOCK = 128

DN_ALPHA = (2 * DEPTH) ** 0.25
DN_BETA = (8 * DEPTH) ** -0.25
LN_EPS = 1e-6
RMS_EPS = 1e-6

kernel_name = 'hybrid_rwkv7_conformer_axialgqa_hmoe'


def _ln(x):
    xf = x.astype(F32)
    mu = jnp.mean(xf, -1, keepdims=True)
    var = jnp.mean(jnp.square(xf - mu), -1, keepdims=True)
    return (xf - mu) * lax.rsqrt(var + LN_EPS)


def _layernorm(x, g, b):
    return (_ln(x) * g + b).astype(x.dtype)


def _modulate(x, shift, scale):
    return (_ln(x) * (1.0 + scale) + shift).astype(x.dtype)


def _adaln(cvec, p):
    m = jax.nn.silu(cvec) @ p['w_mod'] + p['b_mod']
    return jnp.split(m, 6, axis=-1)


def _heads(t, n):
    return t.reshape(*t.shape[:-1], n, t.shape[-1] // n)


def _rms(t, g):
    tf = t.astype(F32)
    return tf * lax.rsqrt(jnp.mean(tf * tf, -1, keepdims=True) + RMS_EPS) * g


def _axial_rope_tables(n_tokens):
    rows = n_tokens // GRID_W
    row = jnp.repeat(jnp.arange(rows, dtype=F32), GRID_W)
    col = jnp.tile(jnp.arange(GRID_W, dtype=F32), rows)
    inv = ROPE_THETA ** (-jnp.arange(ROPE_FREQS, dtype=F32) / ROPE_FREQS)
    ang = jnp.concatenate([row[:, None] * inv, col[:, None] * inv], -1)
    return jnp.cos(ang)[:, None, :], jnp.sin(ang)[:, None, :]


def _rot(t, cos, sin):
    t1, t2 = jnp.split(t, 2, -1)
    return jnp.concatenate([t1 * cos - t2 * sin, t2 * cos + t1 * sin], -1)


def _apply_axial_rope(t, cos, sin):
    tr, tc = jnp.split(t, 2, -1)
    cr, cc = jnp.split(cos, 2, -1)
    sr, sc = jnp.split(sin, 2, -1)
    return jnp.concatenate([_rot(tr, cr, sr), _rot(tc, cc, sc)], -1)


def _gqa(q):
    return q.reshape(*q.shape[:2], ATT_KV_HEADS, ATT_HEADS // ATT_KV_HEADS, ATT_HEAD_DIM)


def _attend(q, k, v):
    s = jnp.einsum('bqhgd,bkhd->bhgqk', q, k)
    w = jax.nn.softmax(s, axis=-1).astype(v.dtype)
    return jnp.einsum('bhgqk,bkhd->bqhgd', w, v)


def _blocked_attend(q, k, v):
    B, T = q.shape[:2]
    nb = T // Q_BLOCK
    qb = jnp.swapaxes(q.reshape(B, nb, Q_BLOCK, *q.shape[2:]), 0, 1)
    ob = lax.map(lambda qi: _attend(qi, k, v), qb)
    return jnp.swapaxes(ob, 0, 1).reshape(q.shape)


def _att_out(o, p):
    return jnp.einsum('btc,cd->btd', o.reshape(*o.shape[:2], ATT_W), p['w_att_o'])


def _token_shift(z, mu):
    prev = jnp.pad(z[:, :-1], ((0, 0), (1, 0), (0, 0)))
    nxt = jnp.pad(z[:, 1:], ((0, 0), (0, 1), (0, 0)))
    return z + mu[0] * (prev - z) + mu[1] * (nxt - z)


def _rwkv_inputs(z, p):
    B, T, _ = z.shape
    z = _token_shift(z.astype(F32), p['rwkv_mu'])
    r, k, v, wl, al, gl = jnp.split(z, RWKV_OFFSETS, axis=-1)
    wl = wl.reshape(B, T, 2, DECAY_RANK)
    al = al.reshape(B, T, 2, ICLR_RANK)
    w = -jax.nn.softplus(-(p['rwkv_w0'] + jnp.einsum('btdr,drc->btdc', jnp.tanh(wl), p['rwkv_w2']))) - 0.5
    decay = jnp.exp(-jnp.exp(w))
    a = jax.nn.sigmoid(p['rwkv_a0'] + jnp.einsum('btdr,drc->btdc', al, p['rwkv_a2']))
    g = jnp.einsum('btr,rc->btc', jax.nn.sigmoid(gl), p['rwkv_g2'])
    kk = _heads(k * p['rwkv_k_k'], RWKV_HEADS)
    kk = kk / jnp.maximum(jnp.sqrt(jnp.sum(kk * kk, -1, keepdims=True)), 1e-12)
    kd = k[:, :, None, :] * (1.0 + (a - 1.0) * p['rwkv_k_a'])
    H = lambda t: _heads(t, RWKV_HEADS)
    return (H(r), H(v), kk, H(kd), H(decay), H(a), g)


def _rwkv_scan(state0, r, v, kk, kd, decay, a):
    def both(t):
        tt = jnp.moveaxis(t, 1, 0)
        return jnp.stack([tt, tt[::-1]], axis=1)

    def per_dir(t):
        tt = jnp.moveaxis(t, 1, 0)
        return jnp.stack([tt[:, :, 0], tt[::-1, :, 1]], axis=1)

    xs = (both(r), both(v), both(kk), per_dir(kd), per_dir(decay), per_dir(kk[:, :, None] * a))

    def step(S, inp):
        r_t, v_t, kk_t, k_t, w_t, b_t = inp
        sa = jnp.einsum('dbhij,dbhj->dbhi', S, -kk_t)
        S = S * w_t[..., None, :] + sa[..., :, None] * b_t[..., None, :] + v_t[..., :, None] * k_t[..., None, :]
        return S, jnp.einsum('dbhij,dbhj->dbhi', S, r_t)

    S, y = lax.scan(step, state0, xs)
    y = y[:, 0] + y[::-1, 1]
    return S, jnp.moveaxis(y, 0, 1)


def _rwkv_out(y, ins, p, dtype):
    r, v, _, kd, _, _, g = ins
    B, T = y.shape[:2]
    mu = jnp.mean(y, -1, keepdims=True)
    var = jnp.mean(jnp.square(y - mu), -1, keepdims=True)
    yn = ((y - mu) * lax.rsqrt(var + GN_EPS)).reshape(B, T, RWKV_W) * p['rwkv_ln_g'] + p['rwkv_ln_b']
    bonus = jnp.sum(r[:, :, None] * kd * p['rwkv_r_k'], -1, keepdims=True) * v[:, :, None]
    yo = (yn + jnp.sum(bonus, 2).reshape(B, T, RWKV_W)) * g
    return jnp.einsum('btc,cd->btd', yo.astype(dtype), p['w_rwkv_o'])


def _conv_module(z, p):
    u = z[..., :CONV_W] * jax.nn.sigmoid(z[..., CONV_W:])
    u = lax.conv_general_dilated(u, p['conv_w'][:, None, :], window_strides=(1,),
                                 padding=((CONV_K // 2, CONV_K // 2),),
                                 dimension_numbers=('NWC', 'WIO', 'NWC'),
                                 feature_group_count=CONV_W) + p['conv_b']
    u = jax.nn.silu(_layernorm(u, p['conv_ln_g'], p['conv_ln_b']))
    return jnp.einsum('btc,cd->btd', u, p['w_conv_o'])


def _merge(gt, ys, p):
    B, T, _ = gt.shape
    g = jax.nn.sigmoid(gt + p['b_gate']).reshape(B, T, N_BRANCH, D_MODEL)
    m = g[:, :, 0] * ys[0] + g[:, :, 1] * ys[1] + g[:, :, 2] * ys[2]
    return jnp.einsum('btd,de->bte', m, p['w_out'])


def _mixer(h, hc, p, need_ctx):
    B, S, _ = h.shape
    dt = h.dtype
    qa, ka, va, rwa, cva, gta = jnp.split(jnp.einsum('btd,de->bte', h, p['w_in']), IN_OFFSETS, -1)
    qc, kc, vc, rwc, cvc, gtc = jnp.split(jnp.einsum('btd,de->bte', hc, p['w_in']), IN_OFFSETS, -1)

    cos, sin = _axial_rope_tables(S)
    q = _apply_axial_rope(_rms(_heads(qa, ATT_HEADS), p['q_norm']), cos, sin) * ATT_SCALE
    k = _apply_axial_rope(_rms(_heads(ka, ATT_KV_HEADS), p['k_norm']), cos, sin)
    k_ctx = _rms(_heads(kc, ATT_KV_HEADS), p['k_norm'])
    v_ctx = _heads(vc, ATT_KV_HEADS)
    k_all = jnp.concatenate([k, k_ctx], 1)
    v_all = jnp.concatenate([_heads(va, ATT_KV_HEADS), v_ctx], 1)
    y_att = _att_out(_blocked_attend(_gqa(q), k_all, v_all), p)

    r_c = _rwkv_inputs(rwc, p)
    state0 = jnp.zeros((2, B, RWKV_HEADS, RWKV_HEAD_DIM, RWKV_HEAD_DIM), F32)
    state_ctx, yr_c = _rwkv_scan(state0, *r_c[:6])
    r_l = _rwkv_inputs(rwa, p)
    _, yr_l = _rwkv_scan(state_ctx, *r_l[:6])
    y_rwkv = _rwkv_out(yr_l, r_l, p, dt)

    y_conv = _conv_module(cva, p)

    y = _merge(gta, (y_att, y_rwkv, y_conv), p)
    if not need_ctx:
        return y, None
    q_c = _rms(_heads(qc, ATT_HEADS), p['q_norm']) * ATT_SCALE
    yc_att = _att_out(_attend(_gqa(q_c), k_ctx, v_ctx), p)
    yc_rwkv = _rwkv_out(yr_c, r_c, p, dt)
    yc_conv = _conv_module(cvc, p)
    yc = _merge(gtc, (yc_att, yc_rwkv, yc_conv), p)
    return y, yc


def _moe(xt, p):
    n, D = xt.shape
    rows = jnp.arange(n)
    hf = xt.astype(F32)
    grp_logits = hf @ p['w_group'] + p['b_group']
    grp = jnp.argmax(grp_logits, -1)
    grp_w = jax.nn.softmax(grp_logits, -1)[rows, grp][:, None]
    exp_logits = (hf @ p['w_router'] + p['b_router']).reshape(n, N_GROUPS, EXPERTS_PER_GROUP)
    in_grp = exp_logits[rows, grp]
    top_p, top_i = lax.top_k(jax.nn.softmax(in_grp, -1), TOP_K)
    wts = grp_w * top_p / jnp.sum(top_p, -1, keepdims=True)
    ids = grp[:, None] * EXPERTS_PER_GROUP + top_i

    A = n * TOP_K
    flat_e = ids.reshape(-1)
    flat_tok = jnp.repeat(rows, TOP_K)
    flat_w = wts.reshape(-1)
    order = jnp.argsort(flat_e)
    se = flat_e[order]
    counts = jnp.bincount(flat_e, length=N_EXPERTS)
    padded = (counts + MOE_BLOCK - 1) // MOE_BLOCK * MOE_BLOCK
    pad_end = jnp.cumsum(padded)
    pad_start = pad_end - padded
    start = jnp.cumsum(counts) - counts
    dest = pad_start[se] + jnp.arange(A) - start[se]
    n_blocks = -(-A // MOE_BLOCK) + N_EXPERTS
    slot_tok = jnp.full((n_blocks * MOE_BLOCK,), n, jnp.int32).at[dest].set(flat_tok[order].astype(jnp.int32))
    slot_w = jnp.zeros((n_blocks * MOE_BLOCK,), F32).at[dest].set(flat_w[order])
    block_e = jnp.minimum(jnp.searchsorted(pad_end, jnp.arange(n_blocks) * MOE_BLOCK, side='right'), N_EXPERTS - 1)
    x_pad = jnp.concatenate([xt, jnp.zeros((1, D), xt.dtype)], 0)

    def run_block(args):
        tok, e = args
        xb = x_pad[tok]
        hdn = jax.nn.silu(xb @ p['w_e_gate'][e]) * (xb @ p['w_e_up'][e])
        return hdn @ p['w_e_down'][e]

    yb = lax.map(run_block, (slot_tok.reshape(n_blocks, MOE_BLOCK), block_e))
    y = jnp.zeros((n + 1, D), F32).at[slot_tok].add(yb.reshape(-1, D).astype(F32) * slot_w[:, None])
    return y[:n].astype(xt.dtype)


def _layer(x, xc, c, c_ctx, p, last):
    B, S, D = x.shape
    sh1, sc1, gm1, sh2, sc2, gm2 = _adaln(c, p)
    csh1, csc1, cgm1, csh2, csc2, cgm2 = _adaln(c_ctx, p)
    h = _modulate(x, sh1[:, None], sc1[:, None])
    hc = _modulate(xc, csh1, csc1)
    y, yc = _mixer(h, hc, p, not last)
    x = _layernorm(DN_ALPHA * x + gm1[:, None] * y, p['ln1_g'], p['ln1_b'])
    h2 = _modulate(x, sh2[:, None], sc2[:, None]).reshape(B * S, D)
    if last:
        f = _moe(h2, p)
    else:
        xc = _layernorm(DN_ALPHA * xc + cgm1 * yc, p['ln1_g'], p['ln1_b'])
        hc2 = _modulate(xc, csh2, csc2).reshape(-1, D)
        f_all = _moe(jnp.concatenate([h2, hc2], 0), p)
        f = f_all[:B * S]
        xc = _layernorm(DN_ALPHA * xc + cgm2 * f_all[B * S:].reshape(xc.shape), p['ln2_g'], p['ln2_b'])
    x = _layernorm(DN_ALPHA * x + gm2[:, None] * f.reshape(B, S, D), p['ln2_g'], p['ln2_b'])
    return x, xc


def setup_inputs(seed: int = 0) -> dict:
    key = jax.random.key(seed)
    ks = iter(jax.random.split(key, 64))
    D, L = D_MODEL, DEPTH

    def nrm(shape, scale):
        return scale * jax.random.normal(next(ks), shape, F32)

    def near_one(shape):
        return 1.0 + nrm(shape, 0.02)

    return {
        'x': nrm((BATCH, SEQ, D), 1.0),
        'c': nrm((BATCH, D), 1.0),
        'ctx': nrm((BATCH, CTX_LEN, D), 1.0),
        'c_ctx': nrm((D,), 1.0),
        'w_mod': nrm((L, D, 6 * D), 0.5 * D ** -0.5),
        'b_mod': nrm((L, 6 * D), 0.01),
        'w_in': nrm((L, D, IN_W), D ** -0.5),
        'b_gate': nrm((L, N_BRANCH * D), 0.01),
        'q_norm': near_one((L, ATT_HEAD_DIM)),
        'k_norm': near_one((L, ATT_HEAD_DIM)),
        'w_att_o': nrm((L, ATT_W, D), ATT_W ** -0.5),
        'rwkv_mu': jax.random.uniform(next(ks), (L, 2, RWKV_IN_W), F32, 0.0, 0.5),
        'rwkv_w0': jax.random.uniform(next(ks), (L, 2, RWKV_W), F32, -6.0, -1.0),
        'rwkv_w2': nrm((L, 2, DECAY_RANK, RWKV_W), 0.1),
        'rwkv_a0': nrm((L, 2, RWKV_W), 0.1),
        'rwkv_a2': nrm((L, 2, ICLR_RANK, RWKV_W), 0.1 * ICLR_RANK ** -0.5),
        'rwkv_g2': nrm((L, GATE_RANK, RWKV_W), GATE_RANK ** -0.5),
        'rwkv_k_k': 0.85 + nrm((L, RWKV_W), 0.02),
        'rwkv_k_a': near_one((L, RWKV_W)),
        'rwkv_r_k': nrm((L, RWKV_HEADS, RWKV_HEAD_DIM), 0.1),
        'rwkv_ln_g': near_one((L, RWKV_W)),
        'rwkv_ln_b': nrm((L, RWKV_W), 0.01),
        'w_rwkv_o': nrm((L, RWKV_W, D), RWKV_W ** -0.5),
        'conv_w': nrm((L, CONV_K, CONV_W), CONV_K ** -0.5),
        'conv_b': nrm((L, CONV_W), 0.01),
        'conv_ln_g': near_one((L, CONV_W)),
        'conv_ln_b': nrm((L, CONV_W), 0.01),
        'w_conv_o': nrm((L, CONV_W, D), CONV_W ** -0.5),
        'w_out': nrm((L, D, D), DN_BETA * D ** -0.5),
        'ln1_g': near_one((L, D)),
        'ln1_b': nrm((L, D), 0.01),
        'w_group': nrm((L, D, N_GROUPS), D ** -0.5),
        'b_group': nrm((L, N_GROUPS), 0.01),
        'w_router': nrm((L, D, N_EXPERTS), D ** -0.5),
        'b_router': nrm((L, N_EXPERTS), 0.01),
        'w_e_gate': nrm((L, N_EXPERTS, D, EXPERT_FF), D ** -0.5),
        'w_e_up': nrm((L, N_EXPERTS, D, EXPERT_FF), D ** -0.5),
        'w_e_down': nrm((L, N_EXPERTS, EXPERT_FF, D), DN_BETA * EXPERT_FF ** -0.5),
        'ln2_g': near_one((L, D)),
        'ln2_b': nrm((L, D), 0.01),
    }


def reference(x, c, ctx, c_ctx, w_mod, b_mod, w_in, b_gate, q_norm, k_norm, w_att_o,
              rwkv_mu, rwkv_w0, rwkv_w2, rwkv_a0, rwkv_a2, rwkv_g2, rwkv_k_k, rwkv_k_a, rwkv_r_k,
              rwkv_ln_g, rwkv_ln_b, w_rwkv_o, conv_w, conv_b, conv_ln_g, conv_ln_b, w_conv_o,
              w_out, ln1_g, ln1_b, w_group, b_group, w_router, b_router, w_e_gate, w_e_up,
              w_e_down, ln2_g, ln2_b):
    xc = ctx
    for l in range(DEPTH):
        p = dict(w_mod=w_mod[l], b_mod=b_mod[l], w_in=w_in[l], b_gate=b_gate[l],
                 q_norm=q_norm[l], k_norm=k_norm[l], w_att_o=w_att_o[l],
                 rwkv_mu=rwkv_mu[l], rwkv_w0=rwkv_w0[l], rwkv_w2=rwkv_w2[l], rwkv_a0=rwkv_a0[l],
                 rwkv_a2=rwkv_a2[l], rwkv_g2=rwkv_g2[l], rwkv_k_k=rwkv_k_k[l], rwkv_k_a=rwkv_k_a[l],
                 rwkv_r_k=rwkv_r_k[l], rwkv_ln_g=rwkv_ln_g[l], rwkv_ln_b=rwkv_ln_b[l],
                 w_rwkv_o=w_rwkv_o[l], conv_w=conv_w[l], conv_b=conv_b[l], conv_ln_g=conv_ln_g[l],
                 conv_ln_b=conv_ln_b[l], w_conv_o=w_conv_o[l], w_out=w_out[l],
                 ln1_g=ln1_g[l], ln1_b=ln1_b[l], w_group=w_group[l], b_group=b_group[l],
                 w_router=w_router[l], b_router=b_router[l], w_e_gate=w_e_gate[l],
                 w_e_up=w_e_up[l], w_e_down=w_e_down[l], ln2_g=ln2_g[l], ln2_b=ln2_b[l])
        x, xc = _layer(x, xc, c, c_ctx, p, l == DEPTH - 1)
    return x
```

```python
import numpy as np
from contextlib import ExitStack, contextmanager
import concourse.bass as bass
import concourse.mybir as mybir
from concourse.bass_utils import run_bass_kernel_spmd

F32 = mybir.dt.float32
BF16 = mybir.dt.bfloat16
ALU = mybir.AluOpType
AF = mybir.ActivationFunctionType
AX = mybir.AxisListType

NDS = 24
D = 2048
KC = 16
S = 2048
CL = 256
T = S + CL
NBL = 2
DEPTH = 2
RIN = 3456
INW = 13184
Q0, K0, V0, R0, C0, G0 = 0, 1024, 1280, 1536, 4992, 7040
NE = 32
FF = 1024
DN_ALPHA = float((2 * DEPTH) ** 0.25)
LN_EPS = 1e-6
RMS_EPS = 1e-6
GN_EPS = 64e-5
ATT_SCALE = 128 ** -0.5
NTB = T // 128
TILES = [(0, 256), (256, 512), (768, 512), (1280, 512), (1792, 512)]
TILES_LAT = TILES[1:]


class Buf:
    __slots__ = ("name", "w", "r")

    def __init__(self, name=""):
        self.name = name
        self.w = {}
        self.r = {}


def _merge(dst, src):
    for k, (sem, val) in src.items():
        if k not in dst or dst[k][1] < val:
            dst[k] = (sem, val)


class KB:
    def __init__(self, nc):
        self.nc = nc
        self.es = ExitStack()
        self.eng = {"pe": nc.tensor, "act": nc.scalar, "dve": nc.vector, "pool": nc.gpsimd, "sp": nc.sync}
        self.sem = {k: self.es.enter_context(nc.semaphore("s_" + k)) for k in self.eng}
        self.cnt = {k: 0 for k in self.eng}
        self.waited = {k: {} for k in self.eng}
        self.dsems = [self.es.enter_context(nc.semaphore(f"dq{i}")) for i in range(NDS)]
        self.dval = [0] * NDS
        self.dnext = 0
        self.nbuf = 0
        self.ninstr = 0

    def buf(self, name=""):
        self.nbuf += 1
        return Buf(name)

    def _wait(self, e, toks):
        for key, (sem, val) in toks.items():
            if key == e and e in ("pe", "sp"):
                continue
            if self.waited[e].get(key, 0) < val:
                self.eng[e].wait_ge(sem, val)
                self.waited[e][key] = val

    def _deps(self, reads, writes, wacc=()):
        toks = {}
        for b in reads:
            _merge(toks, b.w)
        for b in writes:
            _merge(toks, b.w)
            _merge(toks, b.r)
        for b in wacc:
            _merge(toks, b.r)
        return toks

    def _post(self, key, tok, reads, writes, wacc=()):
        for b in writes:
            b.w = {key: tok}
            b.r = {}
        for b in wacc:
            b.w[key] = tok
        for b in reads:
            if b not in writes:
                b.r[key] = tok

    def op(self, e, fn, R=(), W=(), WA=()):
        self._wait(e, self._deps(R, W, WA))
        ins = fn(self.eng[e])
        self.cnt[e] += 1
        self.ninstr += 1
        ins.then_inc(self.sem[e], 1)
        self._post(e, (self.sem[e], self.cnt[e]), R, W, WA)
        return ins

    def mm(self, fns, R=(), W=()):
        e = "pe"
        self._wait(e, self._deps(R, W))
        ins = None
        for fn in fns:
            ins = fn(self.eng[e])
            self.ninstr += 1
        self.cnt[e] += 1
        ins.then_inc(self.sem[e], 1)
        self._post(e, (self.sem[e], self.cnt[e]), R, W)

    def dma(self, out, in_, R=(), W=(), WA=(), q="sp"):
        i = self.dnext
        self.dnext = (i + 1) % NDS
        toks = self._deps(R, W, WA)
        key = ("d", i)
        if self.dval[i]:
            _merge(toks, {key: (self.dsems[i], self.dval[i])})
        self._wait(q, toks)
        ins = self.eng[q].dma_start(out=out, in_=in_)
        self.ninstr += 1
        self.dval[i] += 16
        ins.then_inc(self.dsems[i], 16)
        self._post(key, (self.dsems[i], self.dval[i]), R, W, WA)

    def all_tokens(self):
        toks = {}
        for e in self.eng:
            if self.cnt[e]:
                toks[e] = (self.sem[e], self.cnt[e])
        for i in range(NDS):
            if self.dval[i]:
                toks[("d", i)] = (self.dsems[i], self.dval[i])
        return toks

    def barrier(self):
        toks = self.all_tokens()
        for e in self.eng:
            self._wait(e, dict(toks))

    def finish(self):
        self._wait("sp", self.all_tokens())

    @contextmanager
    def scope(self):
        es = ExitStack()
        try:
            yield Scope(self, es)
        finally:
            self.barrier()
            es.close()

    def tt(self, e, out, in0, in1, op, R=(), W=()):
        return self.op(e, lambda g: g.tensor_tensor(out=out, in0=in0, in1=in1, op=op), R, W)

    def ts(self, e, out, in0, s1, op0, s2=None, op1=None, R=(), W=()):
        if op1 is None:
            return self.op(e, lambda g: g.tensor_scalar(out=out, in0=in0, scalar1=s1, scalar2=None, op0=op0), R, W)
        return self.op(e, lambda g: g.tensor_scalar(out=out, in0=in0, scalar1=s1, scalar2=s2, op0=op0, op1=op1), R, W)

    def stt(self, e, out, in0, scalar, in1, op0, op1, R=(), W=()):
        return self.op(e, lambda g: g.scalar_tensor_tensor(out=out, in0=in0, scalar=scalar, in1=in1, op0=op0, op1=op1), R, W)

    def act(self, out, in_, func, R=(), W=(), **kw):
        return self.op("act", lambda g: g.activation(out=out, in_=in_, func=func, **kw), R, W)

    def cp(self, e, out, in_, R=(), W=()):
        if e == "act":
            return self.op(e, lambda g: g.copy(out=out, in_=in_), R, W)
        return self.op(e, lambda g: g.tensor_copy(out=out, in_=in_), R, W)

    def red(self, out, in_, op, R=(), W=()):
        return self.op("dve", lambda g: g.tensor_reduce(out=out, in_=in_, axis=AX.X, op=op), R, W)


class Scope:
    def __init__(self, kb, es):
        self.kb = kb
        self.es = es

    def sb(self, shape, dtype, name="t", nb=1):
        self.kb.nbuf += 1
        t = self.es.enter_context(self.kb.nc.sbuf_tensor(f"{name}_{self.kb.nbuf}", list(shape), dtype))
        if nb == 1:
            return t, Buf(name)
        return t, [Buf(name) for _ in range(nb)]

    def ps(self, shape=(128, 512), dtype=F32, name="p"):
        self.kb.nbuf += 1
        t = self.es.enter_context(self.kb.nc.psum_tensor(f"{name}_{self.kb.nbuf}", list(shape), dtype))
        return t, Buf(name)


class Ctx:
    pass


def pm(ap, p=128):
    return ap.rearrange("(kc p) n -> p kc n", p=p)


def build(nlayers=DEPTH, dbg=(), small=False):
    nc = bass.Bass("TRN2", target_bir_lowering=False)
    es = ExitStack()
    es.enter_context(nc.allow_non_contiguous_dma("small param layouts"))
    es.enter_context(nc.allow_low_precision("bf16 matmul operands, fp32 accumulation"))

    def din(name, shape, dt=F32):
        return nc.dram_tensor(name, list(shape), dt, kind="ExternalInput").ap()

    def dscr(name, shape, dt=F32):
        kind = "ExternalOutput" if name in dbg else "Internal"
        return nc.dram_tensor(name, list(shape), dt, kind=kind).ap()

    I = Ctx()
    I.x = din("x", [NBL, S, D])
    I.ctx = din("ctx", [NBL, CL, D])
    I.cT = din("cT", [128, KC * 3])
    I.w_mod = din("w_mod", [DEPTH, D, 6 * D])
    I.b_mod = din("b_mod", [DEPTH, 128, 96])
    I.w_in = din("w_in", [DEPTH, D, INW])
    I.b_gate = din("b_gate", [DEPTH, 128, 48])
    I.qk_norm = din("qk_norm", [DEPTH, 128, 2])
    I.w_att_o = din("w_att_o", [DEPTH, 1024, D])
    I.rwkv_mu = din("rwkv_mu", [DEPTH, 2, RIN])
    I.rwkv_w0 = din("rwkv_w0", [DEPTH, 2, 1024])
    I.rwkv_w2 = din("rwkv_w2", [DEPTH, 128, 1024])
    I.rwkv_a0 = din("rwkv_a0", [DEPTH, 2, 1024])
    I.rwkv_a2 = din("rwkv_a2", [DEPTH, 128, 1024])
    I.rwkv_g2 = din("rwkv_g2", [DEPTH, 128, 1024])
    I.rwkv_vecs = din("rwkv_vecs", [DEPTH, 5, 1024])
    I.w_rwkv_o = din("w_rwkv_o", [DEPTH, 1024, D])
    I.conv_w = din("conv_w", [DEPTH, 128, 8, 31])
    I.conv_vecs = din("conv_vecs", [DEPTH, 128, 3, 8])
    I.w_conv_o = din("w_conv_o", [DEPTH, 1024, D])
    I.w_out = din("w_out", [DEPTH, D, D])
    I.ln_vecs = din("ln_vecs", [DEPTH, 128, 4, KC])
    I.w_gr = din("w_gr", [DEPTH, D, 36])
    I.b_gr = din("b_gr", [DEPTH, 36])
    ned = 1 if small else NE
    I.w_e_gate = din("w_e_gate", [DEPTH, ned, D, FF])
    I.w_e_up = din("w_e_up", [DEPTH, ned, D, FF])
    I.w_e_down = din("w_e_down", [DEPTH, ned, FF, D])
    I.consts = din("consts", [128, 5 * 128])
    I.cos = din("cos", [128, T])
    I.sin = din("sin", [128, T])
    out = nc.dram_tensor("out", [NBL, S, D], F32, kind="ExternalOutput").ap()

    Z = Ctx()
    Z.XT = [dscr(f"XT{b}", [D, T]) for b in range(NBL)]
    Z.RT = [dscr(f"RT{b}", [D, T]) for b in range(NBL)]
    Z.QT = [dscr(f"QT{b}", [1024, T], BF16) for b in range(NBL)]
    Z.ZR = [dscr(f"ZR{b}", [T, RIN]) for b in range(NBL)]
    Z.CU = [dscr(f"CU{b}", [1024, T]) for b in range(NBL)]
    Z.G = [dscr(f"G{b}", [3 * D, T], BF16) for b in range(NBL)]
    Z.ATT = [dscr(f"ATT{b}", [1024, T], BF16) for b in range(NBL)]
    Z.RWO = [dscr(f"RWO{b}", [1024, T], BF16) for b in range(NBL)]
    Z.CVO = [dscr(f"CVO{b}", [1024, T], BF16) for b in range(NBL)]
    Z.MT = [dscr(f"MT{b}", [D, T], BF16) for b in range(NBL)]
    Z.H2 = [dscr(f"H2{b}", [D, T], BF16) for b in range(NBL)]
    Z.WTT = dscr("WTT", [NE, NBL * T])
    Z.SC = dscr("SC", [2 * NBL * 16, T, 384])
    Z.YS = dscr("YS", [2 * NBL * 16, T, 64])
    Z.AUX = [dscr(f"AUX{b}", [T, 2, 1024]) for b in range(NBL)]
    Z.RK = [dscr(f"RK{b}", [2, T, 16]) for b in range(NBL)]
    Z.bufs = {}
    if "HTD0" in dbg:
        Z.HTD = [dscr(f"HTD{b}", [D, T]) for b in range(NBL)]
    if "MODD" in dbg:
        Z.MODD = dscr("MODD", [128, DEPTH * 96 * 3])

    def zb(ap_name):
        if ap_name not in Z.bufs:
            Z.bufs[ap_name] = Buf(ap_name)
        return Z.bufs[ap_name]

    kb = KB(nc)
    C = Ctx()
    top = ExitStack()
    tsc = Scope(kb, top)
    cst, cstb = tsc.sb([128, 5 * 128], F32, "consts")
    kb.dma(cst[:], I.consts[:, :], W=[cstb])
    C.cst, C.cstb = cst, cstb
    C.ident, C.J, C.ones_f, C.PT = (cst[:, i_ * 128:(i_ + 1) * 128] for i_ in range(4))
    ones_b, ones_bb = tsc.sb([128, 128], BF16, "ones_b")
    kb.cp("dve", ones_b[:], cst[:, 256:384], R=[cstb], W=[ones_bb])
    C.ones_b, C.ones_bb = ones_b, ones_bb
    MOD, MODb = tsc.sb([128, DEPTH, 96, 3], F32, "MOD")
    C.MOD, C.MODb = MOD, MODb
    stop = [d_ for d_ in dbg if d_.startswith("stop")]
    stop = stop[0] if stop else ""

    phase0(kb, C, I, Z, zb)
    phaseA(kb, C, I, nlayers)
    if hasattr(Z, "MODD"):
        kb.dma(Z.MODD[:, :], C.MOD[:].rearrange("p l j r -> p (l j r)"), R=[C.MODb])
    for l in range(nlayers):
        last = (l == DEPTH - 1)
        for b in range(NBL):
            phaseB(kb, C, I, Z, zb, l, b, last)
        if stop == f"stopB{l}":
            break
        phaseD(kb, C, I, Z, zb, l)
        if stop == f"stopD{l}":
            break
        phaseE(kb, C, I, Z, zb, l)
        if stop == f"stopE{l}":
            break
        for b in range(NBL):
            phaseF(kb, C, I, Z, zb, l, b, last)
        if stop == f"stopF{l}":
            break
        for b in range(NBL):
            phaseG(kb, C, I, Z, zb, l, b, last)
        if stop == f"stopG{l}":
            break
        for b in range(NBL):
            phaseH(kb, C, I, Z, zb, l, b, last)
            phaseI(kb, C, I, Z, zb, l, b, last)
            phaseJ(kb, C, I, Z, zb, l, b, last)
        if stop == f"stopJ{l}":
            break
        phaseK(kb, C, I, Z, zb, l, last)
    phaseFinal(kb, C, I, Z, zb, out)
    kb.finish()
    top.close()
    print("instructions:", kb.ninstr, flush=True)
    return nc


def ln_stats(kb, C, x, xbs, nk, W, sq, sqbs, ps1, ps1b, ps2, ps2b, mean, meanb, rstd, rstdb, nmr, nmrb, dn, epscol):
    for kc in range(nk):
        if kc % 2 == 0:
            kb.act(sq[:, kc, :W], x[:, kc, :W], AF.Square, R=[xbs[kc]], W=[sqbs[kc]])
        else:
            kb.tt("pool", sq[:, kc, :W], x[:, kc, :W], x[:, kc, :W], ALU.mult, R=[xbs[kc]], W=[sqbs[kc]])
    kb.mm([lambda e, kc=kc: e.matmul(ps1[:, :W], lhsT=C.ones_f, rhs=x[:, kc, :W], start=(kc == 0), stop=(kc == nk - 1))
           for kc in range(nk)], R=list(xbs[:nk]) + [C.cstb], W=[ps1b])
    kb.mm([lambda e, kc=kc: e.matmul(ps2[:, :W], lhsT=C.ones_f, rhs=sq[:, kc, :W], start=(kc == 0), stop=(kc == nk - 1))
           for kc in range(nk)], R=list(sqbs[:nk]) + [C.cstb], W=[ps2b])
    kb.act(mean[:, :W], ps1[:, :W], AF.Copy, R=[ps1b], W=[meanb], scale=1.0 / dn)
    kb.tt("dve", rstd[:, :W], mean[:, :W], mean[:, :W], ALU.mult, R=[meanb], W=[rstdb])
    kb.stt("dve", rstd[:, :W], ps2[:, :W], 1.0 / dn, rstd[:, :W], ALU.mult, ALU.subtract, R=[ps2b, rstdb], W=[rstdb])
    kb.ts("dve", rstd[:, :W], rstd[:, :W], 0.0, ALU.max, R=[rstdb], W=[rstdb])
    kb.act(rstd[:, :W], rstd[:, :W], AF.Sqrt, R=[rstdb, C.cstb], W=[rstdb], bias=C.cst[:, 512 + epscol:513 + epscol], scale=1.0)
    kb.op("dve", lambda g: g.reciprocal(out=rstd[:, :W], in_=rstd[:, :W]), R=[rstdb], W=[rstdb])
    kb.stt("dve", nmr[:, :W], mean[:, :W], -1.0, rstd[:, :W], ALU.mult, ALU.mult, R=[meanb, rstdb], W=[nmrb])


def ln_apply(kb, x, xbs, nk, W, rstd, rstdb, nmr, nmrb, out_of, outbs, a_of, b_of, extraR=()):
    for kc in range(nk):
        e = "dve" if kc % 2 == 0 else "pool"
        t = x[:, kc, :W]
        kb.tt(e, t, t, rstd[:, :W], ALU.mult, R=[rstdb], W=[xbs[kc]])
        kb.tt(e, t, t, nmr[:, :W], ALU.add, R=[nmrb], W=[xbs[kc]])
        kb.ts(e, out_of(kc), t, a_of(kc), ALU.mult, b_of(kc), ALU.add, R=[xbs[kc]] + list(extraR), W=[outbs[kc]])


def phase0(kb, C, I, Z, zb):
    with kb.scope() as sc:
        xin = [sc.sb([128, D], F32, "xin") for _ in range(2)]
        xo = [sc.sb([128, KC, 128], F32, "xo", nb=4) for _ in range(2)]
        pst = [sc.ps() for _ in range(4)]
        n = 0
        for b in range(NBL):
            xtv = pm(Z.XT[b])
            for tb in range(NTB):
                src = I.ctx[b, tb * 128:(tb + 1) * 128, :] if tb < 2 else I.x[b, (tb - 2) * 128:(tb - 1) * 128, :]
                xi, xib = xin[n % 2]
                o, obs = xo[n % 2]
                n += 1
                kb.dma(xi[:], src, W=[xib])
                for g in range(4):
                    p, pb = pst[g]
                    kb.mm([lambda e, j=j, g=g, p=p, xi=xi: e.transpose(p[:, j * 128:(j + 1) * 128], xi[:, (g * 4 + j) * 128:(g * 4 + j + 1) * 128], C.ident)
                           for j in range(4)], R=[xib, C.cstb], W=[pb])
                    kb.cp("act" if g % 2 == 0 else "dve", o[:, g * 4:(g + 1) * 4, :], p[:, :].rearrange("p (a t) -> p a t", t=128), R=[pb], W=[obs[g]])
                kb.dma(xtv[:, :, tb * 128:(tb + 1) * 128], o[:], R=obs, WA=[zb(f"XT{b}")])


def phaseFinal(kb, C, I, Z, zb, out):
    with kb.scope() as sc:
        xin = [sc.sb([128, KC, 128], F32, "fin") for _ in range(2)]
        xo = [sc.sb([128, D], F32, "fo", nb=4) for _ in range(2)]
        pst = [sc.ps() for _ in range(4)]
        n = 0
        for b in range(NBL):
            xtv = pm(Z.XT[b])
            for tb in range(2, NTB):
                xi, xib = xin[n % 2]
                o, obs = xo[n % 2]
                n += 1
                kb.dma(xi[:], xtv[:, :, tb * 128:(tb + 1) * 128], R=[zb(f"XT{b}")], W=[xib])
                for g in range(4):
                    p, pb = pst[g]
                    kb.mm([lambda e, j=j, g=g, p=p, xi=xi: e.transpose(p[:, j * 128:(j + 1) * 128], xi[:, g * 4 + j, :], C.ident)
                           for j in range(4)], R=[xib, C.cstb], W=[pb])
                    kb.cp("act" if g % 2 == 0 else "dve", o[:, g * 512:(g + 1) * 512], p[:, :], R=[pb], W=[obs[g]])
                kb.dma(out[b, (tb - 2) * 128:(tb - 1) * 128, :], o[:], R=obs)


def phaseA(kb, C, I, nlayers):
    with kb.scope() as sc:
        cT, cTb = sc.sb([128, KC, 3], F32, "cT")
        kb.dma(cT[:], I.cT.rearrange("p (k r) -> p k r", r=3), W=[cTb])
        sg, sgb = sc.sb([128, KC, 3], F32, "sg")
        kb.act(sg[:], cT[:], AF.Silu, R=[cTb], W=[sgb])
        wts = [sc.sb([128, KC, 512], F32, "wmod") for _ in range(2)]
        bm, bmb = sc.sb([128, 96], F32, "bm")
        ps, psb = sc.ps()
        for l in range(nlayers):
            kb.dma(bm[:], I.b_mod[l], W=[bmb])
            for g in range(24):
                w, wb = wts[g % 2]
                kb.dma(w[:], pm(I.w_mod[l, :, g * 512:(g + 1) * 512]), W=[wb])
                for j in range(4):
                    mc = g * 4 + j
                    kb.mm([lambda e, kc=kc, j=j, w=w, mc=mc: e.matmul(ps[:, mc * 3:(mc + 1) * 3], lhsT=w[:, kc, j * 128:(j + 1) * 128], rhs=sg[:, kc, :],
                                                                   start=(kc == 0), stop=(kc == KC - 1)) for kc in range(KC)], R=[wb, sgb], W=[psb])
            kb.tt("dve", C.MOD[:, l, :, :], ps[:, 0:288].rearrange("p (j r) -> p j r", r=3), bm[:].unsqueeze(2).to_broadcast([128, 96, 3]), ALU.add,
                  R=[psb, bmb], W=[C.MODb])
            for (a, b_) in ((16, 32), (64, 80)):
                kb.ts("dve", C.MOD[:, l, a:b_, :], C.MOD[:, l, a:b_, :], 1.0, ALU.add, R=[C.MODb], W=[C.MODb])


def win_groups():
    gs = [(0, 512, "q", 0), (512, 512, "q", 4), (K0, 256, "k", 0), (V0, 256, "v", 0)]
    for i in range(6):
        gs.append((R0 + i * 512, 512, "r", i * 512))
    gs.append((R0 + 3072, 384, "r", 3072))
    for i in range(4):
        gs.append((C0 + i * 256, 512, "c", i))
    for i in range(12):
        gs.append((G0 + i * 512, 512, "g", i))
    return gs


def phaseB(kb, C, I, Z, zb, l, b, last):
    tiles = TILES
    with kb.scope() as scB:
        KT, KTb = scB.sb([128, 2, T], BF16, "KT")
        VT, VTb = scB.sb([128, NTB, 256], BF16, "VT")
        with kb.scope() as scH:
            HT, HTbs = scH.sb([128, KC, T], BF16, "HT", nb=KC)
            with kb.scope() as sc:
                xs = [sc.sb([128, KC, 256], F32, "xs", nb=KC) for _ in range(2)]
                sq, sqbs = sc.sb([128, KC, 256], F32, "sq", nb=KC)
                ps1, ps1b = sc.ps()
                ps2, ps2b = sc.ps()
                mean, meanb = sc.sb([128, 256], F32, "mean")
                rstd, rstdb = sc.sb([128, 256], F32, "rstd")
                nmr, nmrb = sc.sb([128, 256], F32, "nmr")
                xtv = pm(Z.XT[b])
                for ti in range(T // 256):
                    t0 = ti * 256
                    x, xbs = xs[ti % 2]
                    r = 2 if t0 < CL else b
                    kb.dma(x[:], xtv[:, :, t0:t0 + 256], R=[zb(f"XT{b}")], W=xbs)
                    ln_stats(kb, C, x, xbs, KC, 256, sq, sqbs, ps1, ps1b, ps2, ps2b, mean, meanb, rstd, rstdb, nmr, nmrb, D, 0)
                    ln_apply(kb, x, xbs, KC, 256, rstd, rstdb, nmr, nmrb, lambda kc: HT[:, kc, t0:t0 + 256], HTbs,
                             lambda kc: C.MOD[:, l, 16 + kc, r:r + 1], lambda kc: C.MOD[:, l, kc, r:r + 1], extraR=[C.MODb])
            if hasattr(Z, "HTD"):
                with kb.scope() as sc:
                    st, stb = sc.sb([128, KC, 256], F32, "st")
                    for ti in range(T // 256):
                        kb.cp("dve", st[:], HT[:, :, ti * 256:(ti + 1) * 256], R=HTbs, W=[stb])
                        kb.dma(pm(Z.HTD[b])[:, :, ti * 256:(ti + 1) * 256], st[:], R=[stb])
            with kb.scope() as sc:
                cos, cosb = sc.sb([128, T], F32, "cos")
                sin, sinb = sc.sb([128, T], F32, "sin")
                kb.dma(cos[:], I.cos[:, :], W=[cosb])
                kb.dma(sin[:], I.sin[:, :], W=[sinb])
                qkn, qknb = sc.sb([128, 2], F32, "qkn")
                kb.dma(qkn[:], I.qk_norm[l], W=[qknb])
                kb.ts("dve", qkn[:], qkn[:], float(128 ** 0.5), ALU.mult, R=[qknb], W=[qknb])
                bg, bgb = sc.sb([128, 48], F32, "bg")
                kb.dma(bg[:], I.b_gate[l], W=[bgb])
                wts = [sc.sb([128, KC, 512], BF16, "win") for _ in range(2)]
                psM = [sc.ps() for _ in range(4)]
                psA, psAb = sc.ps()
                psB, psBb = sc.ps()
                sq16 = [sc.sb([128, 512], BF16, "sq16") for _ in range(2)]
                r1s = [sc.sb([128, 512], F32, "r1") for _ in range(2)]
                qns = [sc.sb([128, 512], F32, "qn") for _ in range(2)]
                t1s = [sc.sb([128, 512], F32, "t1") for _ in range(2)]
                st16 = [sc.sb([128, 512], BF16, "st16") for _ in range(3)]
                st32 = [sc.sb([128, 512], F32, "st32") for _ in range(3)]
                cnt = {"m": 0, "e": 0, "s16": 0, "s32": 0}

                def gemm_fm(w, wb, j, t0, W_):
                    p, pb = psM[cnt["m"] % 4]
                    cnt["m"] += 1
                    kb.mm([lambda e, kc=kc: e.matmul(p[:, :W_], lhsT=w[:, kc, j * 128:(j + 1) * 128], rhs=HT[:, kc, t0:t0 + W_],
                                                     start=(kc == 0), stop=(kc == KC - 1)) for kc in range(KC)], R=[wb] + HTbs, W=[pb])
                    return p, pb

                def gemm_tm(w, wb, tb, n):
                    p, pb = psM[cnt["m"] % 4]
                    cnt["m"] += 1
                    kb.mm([lambda e, kc=kc: e.matmul(p[:, :n], lhsT=HT[:, kc, tb * 128:(tb + 1) * 128], rhs=w[:, kc, 0:n],
                                                     start=(kc == 0), stop=(kc == KC - 1)) for kc in range(KC)], R=[wb] + HTbs, W=[pb])
                    return p, pb

                for gi, (c0, n, kind, aux) in enumerate(win_groups()):
                    w, wb = wts[gi % 2]
                    if kind == "c":
                        kb.dma(w[:, :, 0:256], pm(I.w_in[l, :, c0:c0 + 256]), W=[wb], q="pool")
                        kb.dma(w[:, :, 256:512], pm(I.w_in[l, :, c0 + 1024:c0 + 1280]), WA=[wb], q="pool")
                    else:
                        kb.dma(w[:, :, 0:n], pm(I.w_in[l, :, c0:c0 + n]), W=[wb], q="pool")
                    if kind in ("q", "k"):
                        for j in range(n // 128):
                            hd = aux + j
                            for (t0, W_) in tiles:
                                if kind == "q" and last and t0 < CL:
                                    continue
                                p, pb = gemm_fm(w, wb, j, t0, W_)
                                i2 = cnt["e"] % 2
                                cnt["e"] += 1
                                s16, s16b = sq16[i2]
                                r1, r1b = r1s[i2]
                                qn, qnb = qns[i2]
                                t1, t1b = t1s[i2]
                                gc = 0 if kind == "q" else 1
                                kb.act(s16[:, :W_], p[:, :W_], AF.Square, R=[pb], W=[s16b])
                                kb.mm([lambda e: e.matmul(psA[:, :W_], lhsT=C.ones_b[:], rhs=s16[:, :W_], start=True, stop=True)], R=[s16b, C.ones_bb], W=[psAb])
                                kb.act(r1[:, :W_], psA[:, :W_], AF.Sqrt, R=[psAb, C.cstb], W=[r1b], bias=C.cst[:, 514:515], scale=1.0)
                                kb.op("dve", lambda g: g.reciprocal(out=r1[:, :W_], in_=r1[:, :W_]), R=[r1b], W=[r1b])
                                kb.stt("dve", qn[:, :W_], p[:, :W_], qkn[:, gc:gc + 1], r1[:, :W_], ALU.mult, ALU.mult, R=[pb, qknb, r1b], W=[qnb])
                                kb.mm([lambda e: e.matmul(psB[:, :W_], lhsT=C.PT, rhs=qn[:, :W_], start=True, stop=True)], R=[qnb, C.cstb], W=[psBb])
                                kb.tt("pool", t1[:, :W_], qn[:, :W_], cos[:, t0:t0 + W_], ALU.mult, R=[qnb, cosb], W=[t1b])
                                kb.tt("dve", qn[:, :W_], psB[:, :W_], sin[:, t0:t0 + W_], ALU.mult, R=[psBb, sinb], W=[qnb])
                                if kind == "k":
                                    kb.tt("dve", KT[:, hd, t0:t0 + W_], t1[:, :W_], qn[:, :W_], ALU.add, R=[t1b, qnb], W=[KTb])
                                else:
                                    o16, o16b = st16[cnt["s16"] % 3]
                                    cnt["s16"] += 1
                                    kb.tt("dve", o16[:, :W_], t1[:, :W_], qn[:, :W_], ALU.add, R=[t1b, qnb], W=[o16b])
                                    kb.dma(Z.QT[b][hd * 128:(hd + 1) * 128, t0:t0 + W_], o16[:, :W_], R=[o16b], WA=[zb(f"QT{b}")])
                    elif kind == "v":
                        for tb in range(NTB):
                            p, pb = gemm_tm(w, wb, tb, 256)
                            kb.cp("act", VT[:, tb, :], p[:, :256], R=[pb], W=[VTb])
                    elif kind == "r":
                        for tb in range(NTB):
                            p, pb = gemm_tm(w, wb, tb, n)
                            o32, o32b = st32[cnt["s32"] % 3]
                            cnt["s32"] += 1
                            kb.cp("act" if tb % 2 == 0 else "dve", o32[:, :n], p[:, :n], R=[pb], W=[o32b])
                            kb.dma(Z.ZR[b][tb * 128:(tb + 1) * 128, aux:aux + n], o32[:, :n], R=[o32b], WA=[zb(f"ZR{b}")])
                    elif kind == "c":
                        for jj in range(2):
                            cc = aux * 2 + jj
                            for (t0, W_) in tiles:
                                if last and t0 < CL:
                                    continue
                                pv, pvb = gemm_fm(w, wb, jj, t0, W_)
                                pg, pgb = gemm_fm(w, wb, 2 + jj, t0, W_)
                                sgt, sgtb = st32[cnt["s32"] % 3]
                                cnt["s32"] += 1
                                kb.act(sgt[:, :W_], pg[:, :W_], AF.Sigmoid, R=[pgb], W=[sgtb])
                                kb.tt("dve", sgt[:, :W_], pv[:, :W_], sgt[:, :W_], ALU.mult, R=[pvb, sgtb], W=[sgtb])
                                kb.dma(Z.CU[b][cc * 128:(cc + 1) * 128, t0:t0 + W_], sgt[:, :W_], R=[sgtb], WA=[zb(f"CU{b}")])
                    elif kind == "g":
                        for j in range(4):
                            mc = aux * 4 + j
                            for (t0, W_) in tiles:
                                if last and t0 < CL:
                                    continue
                                p, pb = gemm_fm(w, wb, j, t0, W_)
                                o16, o16b = st16[cnt["s16"] % 3]
                                cnt["s16"] += 1
                                kb.act(o16[:, :W_], p[:, :W_], AF.Sigmoid, R=[pb, bgb], W=[o16b], bias=bg[:, mc:mc + 1], scale=1.0)
                                kb.dma(Z.G[b][mc * 128:(mc + 1) * 128, t0:t0 + W_], o16[:, :W_], R=[o16b], WA=[zb(f"G{b}")])
        with kb.scope() as sc:
            qts = [sc.sb([128, 512], BF16, "qt") for _ in range(2)]
            pTs = [sc.sb([128, 512], BF16, "pT") for _ in range(3)]
            psS = [sc.ps() for _ in range(3)]
            psO = [sc.ps() for _ in range(2)]
            psL = [sc.ps() for _ in range(2)]
            rls = [sc.sb([128, 512], F32, "rl") for _ in range(2)]
            ats = [sc.sb([128, 512], BF16, "at") for _ in range(2)]
            n = 0
            m = 0
            for h in range(8):
                g = h // 4
                for (t0, W_) in tiles:
                    if t0 < CL and last:
                        continue
                    kbl = [0, 1] if t0 < CL else list(range(NTB))
                    qt, qtb = qts[n % 2]
                    po, pob = psO[n % 2]
                    pl, plb = psL[n % 2]
                    rl, rlb = rls[n % 2]
                    at, atb = ats[n % 2]
                    n += 1
                    kb.dma(qt[:, :W_], Z.QT[b][h * 128:(h + 1) * 128, t0:t0 + W_], R=[zb(f"QT{b}")], W=[qtb])
                    for ki, kblk in enumerate(kbl):
                        pS, pSb = psS[m % 3]
                        pT, pTb = pTs[m % 3]
                        m += 1
                        kb.mm([lambda e: e.matmul(pS[:, :W_], lhsT=KT[:, g, kblk * 128:(kblk + 1) * 128], rhs=qt[:, :W_], start=True, stop=True)],
                              R=[KTb, qtb], W=[pSb])
                        kb.act(pT[:, :W_], pS[:, :W_], AF.Exp, R=[pSb], W=[pTb], scale=float(ATT_SCALE))
                        kb.mm([lambda e: e.matmul(po[:, :W_], lhsT=VT[:, kblk, g * 128:(g + 1) * 128], rhs=pT[:, :W_], start=(ki == 0), stop=(ki == len(kbl) - 1))],
                              R=[VTb, pTb], W=[pob])
                        kb.mm([lambda e: e.matmul(pl[:, :W_], lhsT=C.ones_b[:], rhs=pT[:, :W_], start=(ki == 0), stop=(ki == len(kbl) - 1))],
                              R=[C.ones_bb, pTb], W=[plb])
                    kb.op("dve", lambda g_: g_.reciprocal(out=rl[:, :W_], in_=pl[:, :W_]), R=[plb], W=[rlb])
                    kb.tt("dve", at[:, :W_], po[:, :W_], rl[:, :W_], ALU.mult, R=[pob, rlb], W=[atb])
                    kb.dma(Z.ATT[b][h * 128:(h + 1) * 128, t0:t0 + W_], at[:, :W_], R=[atb], WA=[zb(f"ATT{b}")])

def sc_row(d, b):
    return (d * NBL + b) * 16


def rev_block(tb):
    return (1 - tb) if tb < 2 else (19 - tb)


def phaseD(kb, C, I, Z, zb, l):
    with kb.scope() as sc:
        bc = lambda ap: ap.partition_broadcast(128)
        MU = [sc.sb([128, RIN], F32, "mu") for _ in range(2)]
        W0 = [sc.sb([128, 1024], F32, "w0") for _ in range(2)]
        A0 = [sc.sb([128, 1024], F32, "a0") for _ in range(2)]
        for i in range(2):
            kb.dma(MU[i][0][:], bc(I.rwkv_mu[l, i, :]), W=[MU[i][1]])
            kb.dma(W0[i][0][:], bc(I.rwkv_w0[l, i, :]), W=[W0[i][1]])
            kb.dma(A0[i][0][:], bc(I.rwkv_a0[l, i, :]), W=[A0[i][1]])
        KK, KKb = sc.sb([128, 1024], F32, "kk")
        KA, KAb = sc.sb([128, 1024], F32, "ka")
        OM, OMb = sc.sb([128, 1024], F32, "om")
        RKt, RKtb = sc.sb([128, 1024], F32, "rkt")
        kb.dma(KK[:], bc(I.rwkv_vecs[l, 0, :]), W=[KKb])
        kb.dma(KA[:], bc(I.rwkv_vecs[l, 1, :]), W=[KAb])
        kb.dma(RKt[:], bc(I.rwkv_vecs[l, 2, :]), W=[RKtb])
        kb.ts("dve", OM[:], KA[:], -1.0, ALU.mult, 1.0, ALU.add, R=[KAb], W=[OMb])
        W2, W2b = sc.sb([128, 1024], BF16, "w2")
        A2, A2b = sc.sb([128, 1024], BF16, "a2")
        G2, G2b = sc.sb([128, 1024], BF16, "g2")
        kb.dma(W2[:], I.rwkv_w2[l], W=[W2b], q="pool")
        kb.dma(A2[:], I.rwkv_a2[l], W=[A2b], q="pool")
        kb.dma(G2[:], I.rwkv_g2[l], W=[G2b], q="pool")
        Zc, Zcb = sc.sb([128, RIN], F32, "zc")
        Zp, Zpb = sc.sb([128, RIN], F32, "zp")
        Zn, Znb = sc.sb([128, RIN], F32, "zn")
        Zr, Zrb = sc.sb([128, RIN], F32, "zr")
        X1, X1b = sc.sb([128, 1024], F32, "x1")
        DEC, DECb = sc.sb([128, 1024], F32, "dec")
        A_, A_b = sc.sb([128, 1024], F32, "a_")
        KKn, KKnb = sc.sb([128, 1024], F32, "kkn")
        Bt, Btb = sc.sb([128, 1024], F32, "bt")
        KD, KDb = sc.sb([128, 1024], F32, "kd")
        TM, TMb = sc.sb([128, 1024], F32, "tm")
        TG, TGb = sc.sb([128, 1024], F32, "tg")
        wlT, wlTb = sc.sb([128, 128], BF16, "wlT")
        alT, alTb = sc.sb([128, 128], BF16, "alT")
        glT, glTb = sc.sb([128, 128], BF16, "glT")
        ssq, ssqb = sc.sb([128, 16], F32, "ssq")
        rk, rkb = sc.sb([128, 16], F32, "rk")
        pJ = [sc.ps() for _ in range(2)]
        pT, pTb = sc.ps()
        pW = [sc.ps() for _ in range(2)]
        pA = [sc.ps() for _ in range(2)]
        v3 = lambda t: t[:].rearrange("p (h j) -> p h j", j=64)
        nj = 0
        for b in range(NBL):
            ZRd = Z.ZR[b]
            zrb = zb(f"ZR{b}")
            for tb in range(NTB):
                t0 = tb * 128
                kb.dma(Zc[:], ZRd[t0:t0 + 128, :], R=[zrb], W=[Zcb])
                if tb in (0, 2):
                    kb.op("pool", lambda g: g.memset(Zp[:], 0.0), W=[Zpb])
                    kb.dma(Zp[1:128, :], ZRd[t0:t0 + 127, :], R=[zrb], W=[Zpb])
                else:
                    kb.dma(Zp[:], ZRd[t0 - 1:t0 + 127, :], R=[zrb], W=[Zpb])
                if tb in (1, NTB - 1):
                    kb.op("pool", lambda g: g.memset(Zn[:], 0.0), W=[Znb])
                    kb.dma(Zn[0:127, :], ZRd[t0 + 1:t0 + 128, :], R=[zrb], W=[Znb])
                else:
                    kb.dma(Zn[:], ZRd[t0 + 1:t0 + 129, :], R=[zrb], W=[Znb])
                kb.tt("pool", Zp[:], Zp[:], Zc[:], ALU.subtract, R=[Zcb], W=[Zpb])
                kb.tt("pool", Zp[:], Zp[:], MU[0][0][:], ALU.mult, R=[MU[0][1]], W=[Zpb])
                kb.tt("dve", Zn[:], Zn[:], Zc[:], ALU.subtract, R=[Zcb], W=[Znb])
                kb.tt("dve", Zn[:], Zn[:], MU[1][0][:], ALU.mult, R=[MU[1][1]], W=[Znb])
                kb.tt("dve", Zc[:], Zc[:], Zp[:], ALU.add, R=[Zpb], W=[Zcb])
                kb.tt("dve", Zc[:], Zc[:], Zn[:], ALU.add, R=[Znb], W=[Zcb])
                for ci in range(7):
                    c0 = ci * 512
                    n = min(512, RIN - c0)
                    p, pb = pJ[nj % 2]
                    nj += 1
                    kb.mm([lambda e: e.matmul(p[:, :n], lhsT=C.J, rhs=Zc[:, c0:c0 + n], start=True, stop=True)], R=[Zcb, C.cstb], W=[pb])
                    if ci == 0:
                        kb.cp("act", Zr[:, c0:c0 + n], p[:, :n], R=[pb], W=[Zrb])
                    else:
                        kb.op("act" if ci % 2 == 0 else "dve",
                              (lambda g: g.copy(out=Zr[:, c0:c0 + n], in_=p[:, :n])) if ci % 2 == 0 else (lambda g: g.tensor_copy(out=Zr[:, c0:c0 + n], in_=p[:, :n])),
                              R=[pb], WA=[Zrb])
                kb.dma(Z.AUX[b][t0:t0 + 128, 1, :], Zc[:, 2048:3072], R=[Zcb], WA=[zb(f"AUX{b}")])
                import os
                DL = int(os.environ.get("DLEVEL", "9"))
                for d in range(2 if DL >= 2 else 0):
                    zs, zsb = (Zc, Zcb) if d == 0 else (Zr, Zrb)
                    s0 = t0 if d == 0 else rev_block(tb) * 128
                    row0 = sc_row(d, b)
                    ntr = 3 if d == 0 else 2
                    kb.mm([lambda e, i=i: e.transpose(pT[:, i * 128:(i + 1) * 128], zs[:, 3072 + i * 128:3200 + i * 128], C.ident) for i in range(ntr)],
                          R=[zsb, C.cstb], W=[pTb])
                    kb.act(wlT[:], pT[:, 0:128], AF.Tanh, R=[pTb], W=[wlTb])
                    kb.cp("act", alT[:], pT[:, 128:256], R=[pTb], W=[alTb])
                    if d == 0:
                        kb.act(glT[:], pT[:, 256:384], AF.Sigmoid, R=[pTb], W=[glTb])
                    for hf in range(2):
                        kb.mm([lambda e: e.matmul(pW[hf][0][:, :], lhsT=wlT[d * 64:(d + 1) * 64, :], rhs=W2[d * 64:(d + 1) * 64, hf * 512:(hf + 1) * 512], start=True, stop=True)],
                              R=[wlTb, W2b], W=[pW[hf][1]])
                        kb.mm([lambda e: e.matmul(pA[hf][0][:, :], lhsT=alT[d * 64:(d + 1) * 64, :], rhs=A2[d * 64:(d + 1) * 64, hf * 512:(hf + 1) * 512], start=True, stop=True)],
                              R=[alTb, A2b], W=[pA[hf][1]])
                    for hf in range(2):
                        sl = slice(hf * 512, (hf + 1) * 512)
                        if hf == 0:
                            kb.tt("dve", X1[:, sl], pW[hf][0][:, :], W0[d][0][:, sl], ALU.add, R=[pW[hf][1], W0[d][1]], W=[X1b])
                            kb.tt("dve", A_[:, sl], pA[hf][0][:, :], A0[d][0][:, sl], ALU.add, R=[pA[hf][1], A0[d][1]], W=[A_b])
                        else:
                            kb.op("dve", lambda g: g.tensor_tensor(out=X1[:, sl], in0=pW[hf][0][:, :], in1=W0[d][0][:, sl], op=ALU.add), R=[pW[hf][1], W0[d][1]], WA=[X1b])
                            kb.op("dve", lambda g: g.tensor_tensor(out=A_[:, sl], in0=pA[hf][0][:, :], in1=A0[d][0][:, sl], op=ALU.add), R=[pA[hf][1], A0[d][1]], WA=[A_b])
                    kb.act(X1[:], X1[:], AF.Sigmoid, R=[X1b], W=[X1b])
                    kb.act(A_[:], A_[:], AF.Sigmoid, R=[A_b], W=[A_b])
                    kb.act(DEC[:], X1[:], AF.Exp, R=[X1b], W=[DECb], scale=float(-np.exp(-0.5)))
                    k_ = zs[:, 1024:2048]
                    r_ = zs[:, 0:1024]
                    if DL < 3:
                        continue
                    kb.tt("pool", KKn[:], k_, KK[:], ALU.mult, R=[zsb, KKb], W=[KKnb])
                    kb.tt("pool", TM[:], KKn[:], KKn[:], ALU.mult, R=[KKnb], W=[TMb])
                    kb.red(ssq[:], v3(TM), ALU.add, R=[TMb], W=[ssqb])
                    kb.act(ssq[:], ssq[:], AF.Sqrt, R=[ssqb], W=[ssqb])
                    kb.ts("dve", ssq[:], ssq[:], 1e-12, ALU.max, R=[ssqb], W=[ssqb])
                    kb.op("dve", lambda g: g.reciprocal(out=ssq[:], in_=ssq[:]), R=[ssqb], W=[ssqb])
                    kb.tt("dve", v3(KKn), v3(KKn), ssq[:].unsqueeze(2).to_broadcast([128, 16, 64]), ALU.mult, R=[ssqb], W=[KKnb])
                    kb.tt("pool", Bt[:], KKn[:], A_[:], ALU.mult, R=[KKnb, A_b], W=[Btb])
                    kb.tt("pool", TM[:], A_[:], KA[:], ALU.mult, R=[A_b, KAb], W=[TMb])
                    kb.tt("pool", TM[:], TM[:], OM[:], ALU.add, R=[OMb], W=[TMb])
                    kb.tt("pool", KD[:], k_, TM[:], ALU.mult, R=[zsb, TMb], W=[KDb])
                    kb.tt("dve", TM[:], r_, RKt[:], ALU.mult, R=[zsb, RKtb, KDb], W=[TMb])
                    kb.tt("dve", TM[:], TM[:], KD[:], ALU.mult, R=[KDb], W=[TMb])
                    kb.red(rk[:], v3(TM), ALU.add, R=[TMb], W=[rkb])
                    if DL < 4:
                        continue
                    kb.dma(Z.RK[b][d, s0:s0 + 128, :], rk[:], R=[rkb], WA=[zb(f"RK{b}")])
                    fields = [(KKn[:], KKnb), (r_, zsb), (DEC[:], DECb), (Bt[:], Btb), (KD[:], KDb), (zs[:, 2048:3072], zsb)]
                    import os
                    for f, (ap, apb) in enumerate(fields):
                        if os.environ.get("SKIPSC"):
                            continue
                        kb.dma(Z.SC[row0:row0 + 16, s0:s0 + 128, f * 64:(f + 1) * 64].rearrange("h t j -> t h j"),
                               ap.rearrange("p (h j) -> p h j", j=64), R=[apb], WA=[zb("SC")])
                    if d == 0:
                        for hf in range(2):
                            kb.mm([lambda e: e.matmul(pW[hf][0][:, :], lhsT=glT[:, :], rhs=G2[:, hf * 512:(hf + 1) * 512], start=True, stop=True)],
                                  R=[glTb, G2b], W=[pW[hf][1]])
                        kb.cp("act", TG[:, 0:512], pW[0][0][:, :], R=[pW[0][1]], W=[TGb])
                        kb.op("act", lambda g: g.copy(out=TG[:, 512:1024], in_=pW[1][0][:, :]), R=[pW[1][1]], WA=[TGb])
                        kb.dma(Z.AUX[b][t0:t0 + 128, 0, :], TG[:], R=[TGb], WA=[zb(f"AUX{b}")])


def phaseE(kb, C, I, Z, zb, l, nsteps=T):
    CH = 32
    with kb.scope() as sc:
        St, Sb = sc.sb([128, 32, 64], F32, "S")
        ops = [sc.sb([128, CH, 320], F32, "ops") for _ in range(2)]
        vhs = [sc.sb([128, CH, 32], F32, "vh") for _ in range(2)]
        ybs = [sc.sb([128, CH, 32], F32, "yb") for _ in range(2)]
        T1, T1b = sc.sb([128, 32, 64], F32, "T1")
        T2, T2b = sc.sb([128, 32, 64], F32, "T2")
        P1s = [sc.sb([128, 32, 64], F32, "P1") for _ in range(2)]
        T4, T4b = sc.sb([128, 32, 64], F32, "T4")
        sa, sab = sc.sb([128, 32], F32, "sa")
        kb.op("dve", lambda g: g.memset(St[:], 0.0), W=[Sb])
        scb = zb("SC")
        step = 0
        for c in range(nsteps // CH):
            s0 = c * CH
            o, ob = ops[c % 2]
            vh, vhb = vhs[c % 2]
            yb, ybb = ybs[c % 2]
            for ih in range(2):
                if ih == 0:
                    kb.dma(o[0:64, :, :], Z.SC[:, s0:s0 + CH, 0:320], R=[scb], W=[ob])
                    kb.dma(vh[0:64, :, :], Z.SC[:, s0:s0 + CH, 320:352], R=[scb], W=[vhb])
                else:
                    kb.dma(o[64:128, :, :], Z.SC[:, s0:s0 + CH, 0:320], R=[scb], WA=[ob])
                    kb.dma(vh[64:128, :, :], Z.SC[:, s0:s0 + CH, 352:384], R=[scb], WA=[vhb])
            for s in range(CH):
                fb = lambda f: o[:, s, f * 64:(f + 1) * 64].unsqueeze(1).to_broadcast([128, 32, 64])
                P1, P1b = P1s[step % 2]
                step += 1
                kb.tt("pool", T4[:], vh[:, s, :].unsqueeze(2).to_broadcast([128, 32, 64]), fb(4), ALU.mult, R=[vhb, ob], W=[T4b])
                kb.tt("pool", P1[:], St[:], fb(2), ALU.mult, R=[Sb, ob], W=[P1b])
                kb.tt("pool", P1[:], P1[:], T4[:], ALU.add, R=[T4b], W=[P1b])
                kb.tt("dve", T1[:], St[:], fb(0), ALU.mult, R=[Sb, ob], W=[T1b])
                kb.red(sa[:], T1[:], ALU.add, R=[T1b], W=[sab])
                kb.tt("dve", T2[:], sa[:].unsqueeze(2).to_broadcast([128, 32, 64]), fb(3), ALU.mult, R=[sab, ob], W=[T2b])
                kb.tt("dve", St[:], P1[:], T2[:], ALU.subtract, R=[P1b, T2b], W=[Sb])
                kb.tt("dve", T1[:], St[:], fb(1), ALU.mult, R=[Sb, ob], W=[T1b])
                if s == 0:
                    kb.red(yb[:, s, :], T1[:], ALU.add, R=[T1b], W=[ybb])
                else:
                    kb.op("dve", lambda g: g.tensor_reduce(out=yb[:, s, :], in_=T1[:], axis=AX.X, op=ALU.add), R=[T1b], WA=[ybb])
            for ih in range(2):
                kb.dma(Z.YS[:, s0:s0 + CH, ih * 32:(ih + 1) * 32], yb[ih * 64:(ih + 1) * 64, :, :], R=[ybb], WA=[zb("YS")])


def phaseF(kb, C, I, Z, zb, l, b, last):
    with kb.scope() as sc:
        bc = lambda ap: ap.partition_broadcast(128)
        LNG, LNGb = sc.sb([128, 1024], F32, "lng")
        LNB, LNBb = sc.sb([128, 1024], F32, "lnb")
        kb.dma(LNG[:], bc(I.rwkv_vecs[l, 3, :]), W=[LNGb])
        kb.dma(LNB[:], bc(I.rwkv_vecs[l, 4, :]), W=[LNBb])
        idb, idbb = sc.sb([128, 128], BF16, "idb")
        kb.cp("dve", idb[:], C.ident, R=[C.cstb], W=[idbb])
        YF, YFb = sc.sb([128, 1024], F32, "yf")
        YR, YRb = sc.sb([128, 1040], F32, "yr")
        RK0, RK0b = sc.sb([128, 16], F32, "rk0")
        AX_, AXb = sc.sb([128, 2, 1024], F32, "aux")
        Y, Yb = sc.sb([128, 1024], F32, "y")
        SQ, SQb = sc.sb([128, 1024], F32, "sq")
        m16, m16b = sc.sb([128, 16], F32, "m16")
        v16, v16b = sc.sb([128, 16], F32, "v16")
        rks, rksb = sc.sb([128, 16], F32, "rks")
        YO, YOb = sc.sb([128, 1024], BF16, "yo")
        stg, stgb = sc.sb([128, 8, 128], BF16, "stg")
        pFa, pFab = sc.ps()
        pFb, pFbb = sc.ps()
        pFc, pFcb = sc.ps()
        pTr, pTrb = sc.ps([128, 1024], BF16)
        v3 = lambda ap: ap.rearrange("p (h j) -> p h j", j=64)
        b16 = lambda t: t[:].unsqueeze(2).to_broadcast([128, 16, 64])
        for tb in range(2 if last else 0, NTB):
            t0 = tb * 128
            s0r = rev_block(tb) * 128
            kb.dma(v3(YF[:]), Z.YS[sc_row(0, b):sc_row(0, b) + 16, t0:t0 + 128, :].rearrange("h t i -> t h i"), R=[zb("YS")], W=[YFb])
            kb.dma(v3(YR[:, 0:1024]), Z.YS[sc_row(1, b):sc_row(1, b) + 16, s0r:s0r + 128, :].rearrange("h t i -> t h i"), R=[zb("YS")], W=[YRb])
            kb.dma(YR[:, 1024:1040], Z.RK[b][1, s0r:s0r + 128, :], R=[zb(f"RK{b}")], WA=[YRb])
            kb.dma(RK0[:], Z.RK[b][0, t0:t0 + 128, :], R=[zb(f"RK{b}")], W=[RK0b])
            kb.dma(AX_[:], Z.AUX[b][t0:t0 + 128, :, :], R=[zb(f"AUX{b}")], W=[AXb])
            kb.mm([lambda e: e.matmul(pFa[:, :], lhsT=C.J, rhs=YR[:, 0:512], start=True, stop=True)], R=[YRb, C.cstb], W=[pFab])
            kb.mm([lambda e: e.matmul(pFb[:, :], lhsT=C.J, rhs=YR[:, 512:1024], start=True, stop=True)], R=[YRb, C.cstb], W=[pFbb])
            kb.mm([lambda e: e.matmul(pFc[:, 0:16], lhsT=C.J, rhs=YR[:, 1024:1040], start=True, stop=True)], R=[YRb, C.cstb], W=[pFcb])
            kb.tt("dve", Y[:, 0:512], YF[:, 0:512], pFa[:, :], ALU.add, R=[YFb, pFab], W=[Yb])
            kb.op("dve", lambda g: g.tensor_tensor(out=Y[:, 512:1024], in0=YF[:, 512:1024], in1=pFb[:, :], op=ALU.add), R=[YFb, pFbb], WA=[Yb])
            kb.tt("dve", rks[:], RK0[:], pFc[:, 0:16], ALU.add, R=[RK0b, pFcb], W=[rksb])
            kb.red(m16[:], v3(Y[:]), ALU.add, R=[Yb], W=[m16b])
            kb.ts("dve", m16[:], m16[:], 1.0 / 64, ALU.mult, R=[m16b], W=[m16b])
            kb.tt("dve", v3(Y[:]), v3(Y[:]), b16(m16), ALU.subtract, R=[m16b], W=[Yb])
            kb.tt("pool", SQ[:], Y[:], Y[:], ALU.mult, R=[Yb], W=[SQb])
            kb.red(v16[:], v3(SQ[:]), ALU.add, R=[SQb], W=[v16b])
            kb.act(v16[:], v16[:], AF.Sqrt, R=[v16b, C.cstb], W=[v16b], bias=C.cst[:, 513:514], scale=1.0 / 64)
            kb.op("dve", lambda g: g.reciprocal(out=v16[:], in_=v16[:]), R=[v16b], W=[v16b])
            kb.tt("dve", v3(Y[:]), v3(Y[:]), b16(v16), ALU.mult, R=[v16b], W=[Yb])
            kb.tt("pool", Y[:], Y[:], LNG[:], ALU.mult, R=[LNGb], W=[Yb])
            kb.tt("pool", Y[:], Y[:], LNB[:], ALU.add, R=[LNBb], W=[Yb])
            kb.tt("dve", v3(SQ[:]), v3(AX_[:, 1, :]), b16(rks), ALU.mult, R=[AXb, rksb], W=[SQb])
            kb.tt("dve", Y[:], Y[:], SQ[:], ALU.add, R=[SQb], W=[Yb])
            kb.tt("dve", YO[:], Y[:], AX_[:, 0, :], ALU.mult, R=[Yb, AXb], W=[YOb])
            kb.mm([lambda e, j=j: e.transpose(pTr[:, j * 128:(j + 1) * 128], YO[:, j * 128:(j + 1) * 128], idb[:]) for j in range(8)],
                  R=[YOb, idbb], W=[pTrb])
            kb.cp("act", stg[:], pTr[:, :].rearrange("p (c t) -> p c t", t=128), R=[pTrb], W=[stgb])
            kb.dma(pm(Z.RWO[b])[:, :, t0:t0 + 128], stg[:], R=[stgb], WA=[zb(f"RWO{b}")])

def phaseG(kb, C, I, Z, zb, l, b, last):
    with kb.scope() as sc:
        CV, CVbs = sc.sb([128, 8, T], F32, "cv", nb=8)
        UH = [sc.sb([128, S + 30], F32, "uh") for _ in range(2)]
        cw, cwb = sc.sb([128, 8, 31], F32, "cw")
        cvec, cvecb = sc.sb([128, 3, 8], F32, "cvec")
        kb.dma(cw[:], I.conv_w[l], W=[cwb])
        kb.dma(cvec[:], I.conv_vecs[l], W=[cvecb])
        segs = [(CL, S)] if last else [(0, CL), (CL, S)]
        n = 0
        for cc in range(8):
            e = "dve"
            for (t0, W_) in segs:
                uh, uhb = UH[n % 2]
                n += 1
                kb.op(e, lambda g: g.memset(uh[:, 0:15], 0.0), W=[uhb])
                kb.op(e, lambda g: g.memset(uh[:, 15 + W_:30 + W_], 0.0), WA=[uhb])
                kb.dma(uh[:, 15:15 + W_], Z.CU[b][cc * 128:(cc + 1) * 128, t0:t0 + W_], R=[zb(f"CU{b}")], W=[uhb])
                dst = CV[:, cc, t0:t0 + W_]
                kb.ts(e, dst, uh[:, 0:W_], cw[:, cc, 0:1], ALU.mult, cvec[:, 0, cc:cc + 1], ALU.add, R=[uhb, cwb, cvecb], W=[CVbs[cc]])
                for k in range(1, 31):
                    kb.stt(e, dst, uh[:, k:k + W_], cw[:, cc, k:k + 1], dst, ALU.mult, ALU.add, R=[uhb, cwb], W=[CVbs[cc]])
        sq, sqbs = sc.sb([128, 8, 256], F32, "sq", nb=8)
        ps1, ps1b = sc.ps()
        ps2, ps2b = sc.ps()
        mean, meanb = sc.sb([128, 256], F32, "mean")
        rstd, rstdb = sc.sb([128, 256], F32, "rstd")
        nmr, nmrb = sc.sb([128, 256], F32, "nmr")
        stg = [sc.sb([128, 8, 256], BF16, "stg", nb=8) for _ in range(2)]
        for ti in range(1 if last else 0, T // 256):
            t0 = ti * 256
            x = CV[:, :, t0:t0 + 256]
            ln_stats(kb, C, x, CVbs, 8, 256, sq, sqbs, ps1, ps1b, ps2, ps2b, mean, meanb, rstd, rstdb, nmr, nmrb, 1024, 0)
            st, stbs = stg[ti % 2]
            for kc in range(8):
                e = "dve" if kc % 2 == 0 else "pool"
                t = x[:, kc, :]
                kb.tt(e, t, t, rstd[:, :], ALU.mult, R=[rstdb], W=[CVbs[kc]])
                kb.tt(e, t, t, nmr[:, :], ALU.add, R=[nmrb], W=[CVbs[kc]])
                kb.act(st[:, kc, :], t, AF.Silu, R=[CVbs[kc], cvecb], W=[stbs[kc]], scale=cvec[:, 1, kc:kc + 1], bias=cvec[:, 2, kc:kc + 1])
            kb.dma(pm(Z.CVO[b])[:, :, t0:t0 + 256], st[:], R=stbs, WA=[zb(f"CVO{b}")])


def phaseH(kb, C, I, Z, zb, l, b, last):
    tiles = TILES_LAT if last else TILES
    with kb.scope() as sc:
        A3 = [sc.sb([128, 8, T], BF16, "a3") for _ in range(3)]
        srcs = [(Z.ATT[b], f"ATT{b}"), (Z.RWO[b], f"RWO{b}"), (Z.CVO[b], f"CVO{b}")]
        c0 = CL if last else 0
        for br in range(3):
            kb.dma(A3[br][0][:, :, c0:T], pm(srcs[br][0])[:, :, c0:T], R=[zb(srcs[br][1])], W=[A3[br][1]])
        Wd = [I.w_att_o, I.w_rwkv_o, I.w_conv_o]
        wts = [[sc.sb([128, 8, 512], BF16, "wbr") for _ in range(3)] for _ in range(2)]
        gts = [sc.sb([128, 3, 512], BF16, "gt") for _ in range(2)]
        tas = [sc.sb([128, 512], F32, "ta") for _ in range(2)]
        tbs = [sc.sb([128, 512], F32, "tb") for _ in range(2)]
        tcs = [sc.sb([128, 512], F32, "tc") for _ in range(2)]
        mts = [sc.sb([128, 512], BF16, "mt") for _ in range(2)]
        pss = [[sc.ps() for _ in range(3)] for _ in range(2)]
        Gv = Z.G[b].rearrange("(br m p) t -> p br m t", br=3, p=128)
        n = 0
        for og in range(4):
            ws = wts[og % 2]
            for br in range(3):
                kb.dma(ws[br][0][:], pm(Wd[br][l, :, og * 512:(og + 1) * 512]), W=[ws[br][1]], q="pool")
            for j in range(4):
                mc = og * 4 + j
                for (t0, W_) in tiles:
                    i2 = n % 2
                    n += 1
                    gt, gtb = gts[i2]
                    kb.dma(gt[:, :, :W_], Gv[:, :, mc, t0:t0 + W_], R=[zb(f"G{b}")], W=[gtb])
                    P = pss[i2]
                    for br in range(3):
                        kb.mm([lambda e, kc=kc, br=br: e.matmul(P[br][0][:, :W_], lhsT=ws[br][0][:, kc, j * 128:(j + 1) * 128], rhs=A3[br][0][:, kc, t0:t0 + W_],
                                                               start=(kc == 0), stop=(kc == 7)) for kc in range(8)], R=[ws[br][1], A3[br][1]], W=[P[br][1]])
                    ta, tab = tas[i2]
                    tb_, tbb = tbs[i2]
                    tc, tcb = tcs[i2]
                    mt, mtb = mts[i2]
                    kb.tt("dve", ta[:, :W_], P[0][0][:, :W_], gt[:, 0, :W_], ALU.mult, R=[P[0][1], gtb], W=[tab])
                    kb.tt("dve", tb_[:, :W_], P[1][0][:, :W_], gt[:, 1, :W_], ALU.mult, R=[P[1][1], gtb], W=[tbb])
                    kb.tt("dve", tc[:, :W_], P[2][0][:, :W_], gt[:, 2, :W_], ALU.mult, R=[P[2][1], gtb], W=[tcb])
                    kb.tt("pool", ta[:, :W_], ta[:, :W_], tb_[:, :W_], ALU.add, R=[tbb], W=[tab])
                    kb.tt("pool", mt[:, :W_], ta[:, :W_], tc[:, :W_], ALU.add, R=[tab, tcb], W=[mtb])
                    kb.dma(Z.MT[b][mc * 128:(mc + 1) * 128, t0:t0 + W_], mt[:, :W_], R=[mtb], WA=[zb(f"MT{b}")])


def phaseI(kb, C, I, Z, zb, l, b, last):
    tiles = TILES_LAT if last else TILES
    with kb.scope() as sc:
        MTr, MTrb = sc.sb([128, KC, T], BF16, "mtr")
        c0 = CL if last else 0
        kb.dma(MTr[:, :, c0:T], pm(Z.MT[b])[:, :, c0:T], R=[zb(f"MT{b}")], W=[MTrb])
        wts = [sc.sb([128, KC, 512], BF16, "wout") for _ in range(2)]
        xts = [sc.sb([128, 512], F32, "xt") for _ in range(3)]
        yas = [sc.sb([128, 512], F32, "ya") for _ in range(3)]
        pss = [sc.ps() for _ in range(4)]
        n = 0
        for og in range(4):
            w, wb = wts[og % 2]
            kb.dma(w[:], pm(I.w_out[l, :, og * 512:(og + 1) * 512]), W=[wb], q="pool")
            for j in range(4):
                mc = og * 4 + j
                for (t0, W_) in tiles:
                    r = 2 if t0 < CL else b
                    p, pb = pss[n % 4]
                    xt, xtb = xts[n % 3]
                    ya, yab = yas[n % 3]
                    n += 1
                    kb.dma(xt[:, :W_], Z.XT[b][mc * 128:(mc + 1) * 128, t0:t0 + W_], R=[zb(f"XT{b}")], W=[xtb])
                    kb.mm([lambda e, kc=kc: e.matmul(p[:, :W_], lhsT=w[:, kc, j * 128:(j + 1) * 128], rhs=MTr[:, kc, t0:t0 + W_],
                                                     start=(kc == 0), stop=(kc == KC - 1)) for kc in range(KC)], R=[wb, MTrb], W=[pb])
                    kb.act(ya[:, :W_], p[:, :W_], AF.Copy, R=[pb, C.MODb], W=[yab], scale=C.MOD[:, l, 32 + mc, r:r + 1])
                    kb.stt("dve", ya[:, :W_], xt[:, :W_], DN_ALPHA, ya[:, :W_], ALU.mult, ALU.add, R=[xtb], W=[yab])
                    kb.dma(Z.RT[b][mc * 128:(mc + 1) * 128, t0:t0 + W_], ya[:, :W_], R=[yab], WA=[zb(f"RT{b}")])


def phaseJ(kb, C, I, Z, zb, l, b, last):
    with kb.scope() as sc:
        xs = [sc.sb([128, KC, 256], F32, "xs", nb=KC) for _ in range(2)]
        x1s = [sc.sb([128, KC, 256], F32, "x1", nb=KC) for _ in range(2)]
        hf, hfbs = sc.sb([128, KC, 256], F32, "hf", nb=KC)
        hb, hbbs = sc.sb([128, KC, 256], BF16, "hb", nb=KC)
        sq, sqbs = sc.sb([128, KC, 256], F32, "sq", nb=KC)
        ps1, ps1b = sc.ps()
        ps2, ps2b = sc.ps()
        psl, pslb = sc.ps()
        pst, pstb = sc.ps()
        mean, meanb = sc.sb([128, 256], F32, "mean")
        rstd, rstdb = sc.sb([128, 256], F32, "rstd")
        nmr, nmrb = sc.sb([128, 256], F32, "nmr")
        lnv, lnvb = sc.sb([128, 4, KC], F32, "lnv")
        kb.dma(lnv[:], I.ln_vecs[l], W=[lnvb])
        wgr, wgrb = sc.sb([128, KC, 36], F32, "wgr")
        kb.dma(wgr[:], pm(I.w_gr[l]), W=[wgrb])
        bgr, bgrb = sc.sb([128, 36], F32, "bgr")
        kb.dma(bgr[:], I.b_gr[l].partition_broadcast(128), W=[bgrb])
        lg, lgb = sc.sb([128, 36], F32, "lg")
        sm = {nm: sc.sb([128, 8], F32, nm) for nm in ("ig", "pe", "oh1", "p2", "oh2", "wt8", "goh", "gex")}
        s1 = {nm: sc.sb([128, 1], F32, nm) for nm in ("gmax", "ngmax", "gsum", "emax", "nemax", "m1", "m2", "den")}
        wts_, wtsb = sc.sb([128, 32], F32, "wts")
        wtT, wtTb = sc.sb([32, 128], F32, "wtT")
        xtv = pm(Z.XT[b])
        rtv = pm(Z.RT[b])
        h2v = pm(Z.H2[b])
        for ti in range(1 if last else 0, T // 256):
            t0 = ti * 256
            r = 2 if t0 < CL else b
            x, xbs = xs[ti % 2]
            x1, x1bs = x1s[ti % 2]
            kb.dma(x[:], rtv[:, :, t0:t0 + 256], R=[zb(f"RT{b}")], W=xbs)
            ln_stats(kb, C, x, xbs, KC, 256, sq, sqbs, ps1, ps1b, ps2, ps2b, mean, meanb, rstd, rstdb, nmr, nmrb, D, 0)
            ln_apply(kb, x, xbs, KC, 256, rstd, rstdb, nmr, nmrb, lambda kc: x1[:, kc, :], x1bs,
                     lambda kc: lnv[:, 0, kc:kc + 1], lambda kc: lnv[:, 1, kc:kc + 1], extraR=[lnvb])
            kb.dma(xtv[:, :, t0:t0 + 256], x1[:], R=x1bs, WA=[zb(f"XT{b}")])
            ln_stats(kb, C, x1, x1bs, KC, 256, sq, sqbs, ps1, ps1b, ps2, ps2b, mean, meanb, rstd, rstdb, nmr, nmrb, D, 0)
            ln_apply(kb, x1, x1bs, KC, 256, rstd, rstdb, nmr, nmrb, lambda kc: hf[:, kc, :], hfbs,
                     lambda kc: C.MOD[:, l, 64 + kc, r:r + 1], lambda kc: C.MOD[:, l, 48 + kc, r:r + 1], extraR=[C.MODb])
            for kc in range(KC):
                kb.cp("act" if kc % 2 == 0 else "pool", hb[:, kc, :], hf[:, kc, :], R=[hfbs[kc]], W=[hbbs[kc]])
            kb.dma(h2v[:, :, t0:t0 + 256], hb[:], R=hbbs, WA=[zb(f"H2{b}")])
            for sub in range(2):
                kb.mm([lambda e, kc=kc: e.matmul(psl[:, 0:36], lhsT=hf[:, kc, sub * 128:(sub + 1) * 128], rhs=wgr[:, kc, :], start=(kc == 0), stop=(kc == KC - 1))
                       for kc in range(KC)], R=list(hfbs) + [wgrb], W=[pslb])
                kb.tt("dve", lg[:], psl[:, 0:36], bgr[:], ALU.add, R=[pslb, bgrb], W=[lgb])
                T_ = lambda nm: sm[nm][0]
                B_ = lambda nm: sm[nm][1]
                t1 = lambda nm: s1[nm][0]
                b1 = lambda nm: s1[nm][1]
                gl = lg[:, 0:4]
                el = lg[:, 4:36].rearrange("p (g e) -> p g e", e=8)
                kb.op("dve", lambda g: g.tensor_reduce(out=t1("gmax")[:], in_=gl, axis=AX.X, op=ALU.max), R=[lgb], W=[b1("gmax")])
                kb.ts("dve", T_("goh")[:, 0:4], gl, t1("gmax")[:, 0:1], ALU.is_equal, R=[lgb, b1("gmax")], W=[B_("goh")])
                kb.ts("dve", t1("ngmax")[:], t1("gmax")[:], -1.0, ALU.mult, R=[b1("gmax")], W=[b1("ngmax")])
                kb.act(T_("gex")[:, 0:4], gl, AF.Exp, R=[lgb, b1("ngmax")], W=[B_("gex")], bias=t1("ngmax")[:, 0:1], scale=1.0)
                kb.red(t1("gsum")[:], T_("gex")[:, 0:4], ALU.add, R=[B_("gex")], W=[b1("gsum")])
                kb.ts("dve", T_("ig")[:], el[:, 0, :], T_("goh")[:, 0:1], ALU.mult, R=[lgb, B_("goh")], W=[B_("ig")])
                for g_ in range(1, 4):
                    kb.stt("dve", T_("ig")[:], el[:, g_, :], T_("goh")[:, g_:g_ + 1], T_("ig")[:], ALU.mult, ALU.add, R=[lgb, B_("goh")], W=[B_("ig")])
                kb.op("dve", lambda g: g.tensor_reduce(out=t1("emax")[:], in_=T_("ig")[:], axis=AX.X, op=ALU.max), R=[B_("ig")], W=[b1("emax")])
                kb.ts("dve", T_("oh1")[:], T_("ig")[:], t1("emax")[:, 0:1], ALU.is_equal, R=[B_("ig"), b1("emax")], W=[B_("oh1")])
                kb.ts("dve", t1("nemax")[:], t1("emax")[:], -1.0, ALU.mult, R=[b1("emax")], W=[b1("nemax")])
                kb.act(T_("pe")[:], T_("ig")[:], AF.Exp, R=[B_("ig"), b1("nemax")], W=[B_("pe")], bias=t1("nemax")[:, 0:1], scale=1.0)
                kb.op("dve", lambda g: g.tensor_reduce(out=t1("m1")[:], in_=T_("pe")[:], axis=AX.X, op=ALU.max), R=[B_("pe")], W=[b1("m1")])
                kb.stt("dve", T_("p2")[:], T_("oh1")[:], -4.0, T_("pe")[:], ALU.mult, ALU.add, R=[B_("oh1"), B_("pe")], W=[B_("p2")])
                kb.op("dve", lambda g: g.tensor_reduce(out=t1("m2")[:], in_=T_("p2")[:], axis=AX.X, op=ALU.max), R=[B_("p2")], W=[b1("m2")])
                kb.ts("dve", T_("oh2")[:], T_("p2")[:], t1("m2")[:, 0:1], ALU.is_equal, R=[B_("p2"), b1("m2")], W=[B_("oh2")])
                kb.tt("dve", T_("oh1")[:], T_("oh1")[:], T_("oh2")[:], ALU.add, R=[B_("oh2")], W=[B_("oh1")])
                kb.tt("dve", T_("wt8")[:], T_("oh1")[:], T_("pe")[:], ALU.mult, R=[B_("oh1"), B_("pe")], W=[B_("wt8")])
                kb.tt("dve", t1("den")[:], t1("m1")[:], t1("m2")[:], ALU.add, R=[b1("m1"), b1("m2")], W=[b1("den")])
                kb.tt("dve", t1("den")[:], t1("den")[:], t1("gsum")[:], ALU.mult, R=[b1("gsum")], W=[b1("den")])
                kb.op("dve", lambda g: g.reciprocal(out=t1("den")[:], in_=t1("den")[:]), R=[b1("den")], W=[b1("den")])
                kb.ts("dve", T_("wt8")[:], T_("wt8")[:], t1("den")[:, 0:1], ALU.mult, R=[b1("den")], W=[B_("wt8")])
                for g_ in range(4):
                    kb.op("dve", lambda g: g.tensor_scalar(out=wts_[:, g_ * 8:(g_ + 1) * 8], in0=T_("wt8")[:], scalar1=T_("goh")[:, g_:g_ + 1], scalar2=None, op0=ALU.mult),
                          R=[B_("wt8"), B_("goh")], W=[wtsb] if g_ == 0 else [], WA=[] if g_ == 0 else [wtsb])
                kb.mm([lambda e: e.transpose(pst[0:32, 0:128], wts_[:, :], C.ident)], R=[wtsb, C.cstb], W=[pstb])
                kb.cp("act", wtT[:], pst[0:32, 0:128], R=[pstb], W=[wtTb])
                col = b * T + t0 + sub * 128
                kb.dma(Z.WTT[:, col:col + 128], wtT[:], R=[wtTb], WA=[zb("WTT")])


def phaseK(kb, C, I, Z, zb, l, last):
    if last:
        passes = [(b, t0, [(0, 512), (512, 512)]) for b in range(NBL) for t0 in (CL, CL + 1024)]
        PW = 1024
    else:
        passes = [(b, t0, [(0, 384), (384, 384), (768, 384)]) for b in range(NBL) for t0 in (0, 1152)]
        PW = 1152
    for (b, t0, subs) in passes:
        r_of = lambda tt: 2 if tt < CL else b
        with kb.scope() as sc:
            Y, Ybs = sc.sb([128, KC, PW], F32, "yacc", nb=KC)
            with kb.scope() as s1:
                H2r, H2rb = s1.sb([128, KC, PW], BF16, "h2r")
                kb.dma(H2r[:], pm(Z.H2[b])[:, :, t0:t0 + PW], R=[zb(f"H2{b}")], W=[H2rb])
                HD, HDbs = s1.sb([128, 8, PW], BF16, "hdn", nb=8)
                wtb, wtbb = s1.sb([128, PW], F32, "wtb")
                wg = [s1.sb([128, KC, 256], BF16, "wg") for _ in range(2)]
                wu = [s1.sb([128, KC, 256], BF16, "wu") for _ in range(2)]
                wd = [s1.sb([128, 8, 512], BF16, "wd") for _ in range(2)]
                sgs = [s1.sb([128, 384 if not last else 512], F32, "sg") for _ in range(2)]
                psg = [s1.ps() for _ in range(2)]
                psu = [s1.ps() for _ in range(2)]
                psd = [s1.ps() for _ in range(2)]
                ng = 0
                nd = 0
                nw = 0
                nwd = 0
                for e_ in range(NE):
                    col = b * T + t0
                    kb.dma(wtb[:], Z.WTT[e_, col:col + PW].partition_broadcast(128), R=[zb("WTT")], W=[wtbb])
                    for fg in range(4):
                        g_, gb_ = wg[nw % 2]
                        u_, ub_ = wu[nw % 2]
                        nw += 1
                        kb.dma(g_[:], pm(I.w_e_gate[l, e_, :, fg * 256:(fg + 1) * 256]), W=[gb_], q="pool")
                        kb.dma(u_[:], pm(I.w_e_up[l, e_, :, fg * 256:(fg + 1) * 256]), W=[ub_], q="pool")
                        for fj in range(2):
                            fc = fg * 2 + fj
                            for si, (o_, W_) in enumerate(subs):
                                pg, pgb = psg[ng % 2]
                                pu, pub = psu[ng % 2]
                                sg, sgb = sgs[ng % 2]
                                ng += 1
                                kb.mm([lambda e, kc=kc: e.matmul(pg[:, :W_], lhsT=g_[:, kc, fj * 128:(fj + 1) * 128], rhs=H2r[:, kc, o_:o_ + W_],
                                                                 start=(kc == 0), stop=(kc == KC - 1)) for kc in range(KC)], R=[gb_, H2rb], W=[pgb])
                                kb.mm([lambda e, kc=kc: e.matmul(pu[:, :W_], lhsT=u_[:, kc, fj * 128:(fj + 1) * 128], rhs=H2r[:, kc, o_:o_ + W_],
                                                                 start=(kc == 0), stop=(kc == KC - 1)) for kc in range(KC)], R=[ub_, H2rb], W=[pub])
                                kb.act(sg[:, :W_], pg[:, :W_], AF.Silu, R=[pgb], W=[sgb])
                                kb.tt("dve", sg[:, :W_], pu[:, :W_], sg[:, :W_], ALU.mult, R=[pub], W=[sgb])
                                if si == 0:
                                    kb.tt("pool", HD[:, fc, o_:o_ + W_], sg[:, :W_], wtb[:, o_:o_ + W_], ALU.mult, R=[sgb, wtbb], W=[HDbs[fc]])
                                else:
                                    kb.op("pool", lambda g: g.tensor_tensor(out=HD[:, fc, o_:o_ + W_], in0=sg[:, :W_], in1=wtb[:, o_:o_ + W_], op=ALU.mult),
                                          R=[sgb, wtbb], WA=[HDbs[fc]])
                    for og in range(4):
                        d_, db_ = wd[nwd % 2]
                        nwd += 1
                        kb.dma(d_[:], pm(I.w_e_down[l, e_, :, og * 512:(og + 1) * 512]), W=[db_], q="pool")
                        for j in range(4):
                            mc = og * 4 + j
                            for si, (o_, W_) in enumerate(subs):
                                pd, pdb = psd[nd % 2]
                                nd += 1
                                kb.mm([lambda e, fc=fc: e.matmul(pd[:, :W_], lhsT=d_[:, fc, j * 128:(j + 1) * 128], rhs=HD[:, fc, o_:o_ + W_],
                                                                 start=(fc == 0), stop=(fc == 7)) for fc in range(8)], R=[db_] + list(HDbs), W=[pdb])
                                if e_ == 0:
                                    if si == 0:
                                        kb.cp("dve", Y[:, mc, o_:o_ + W_], pd[:, :W_], R=[pdb], W=[Ybs[mc]])
                                    else:
                                        kb.op("dve", lambda g: g.tensor_copy(out=Y[:, mc, o_:o_ + W_], in_=pd[:, :W_]), R=[pdb], WA=[Ybs[mc]])
                                else:
                                    kb.tt("dve", Y[:, mc, o_:o_ + W_], Y[:, mc, o_:o_ + W_], pd[:, :W_], ALU.add, R=[pdb], W=[Ybs[mc]])
            with kb.scope() as s2:
                xt, xtbs = s2.sb([128, KC, 256], F32, "x2", nb=KC)
                sq, sqbs = s2.sb([128, KC, 256], F32, "sq", nb=KC)
                ps1, ps1b = s2.ps()
                ps2, ps2b = s2.ps()
                mean, meanb = s2.sb([128, 256], F32, "mean")
                rstd, rstdb = s2.sb([128, 256], F32, "rstd")
                nmr, nmrb = s2.sb([128, 256], F32, "nmr")
                lnv, lnvb = s2.sb([128, 4, KC], F32, "lnv")
                kb.dma(lnv[:], I.ln_vecs[l], W=[lnvb])
                xtv = pm(Z.XT[b])
                o_ = 0
                while o_ < PW:
                    W_ = min(256, PW - o_)
                    if t0 + o_ < CL < t0 + o_ + W_:
                        W_ = CL - (t0 + o_)
                    r = r_of(t0 + o_)
                    kb.dma(xt[:, :, :W_], xtv[:, :, t0 + o_:t0 + o_ + W_], R=[zb(f"XT{b}")], W=xtbs)
                    for kc in range(KC):
                        e = "dve" if kc % 2 == 0 else "pool"
                        yv = Y[:, kc, o_:o_ + W_]
                        kb.ts(e, yv, yv, C.MOD[:, l, 80 + kc, r:r + 1], ALU.mult, R=[C.MODb], W=[Ybs[kc]])
                        kb.ts(e, xt[:, kc, :W_], xt[:, kc, :W_], DN_ALPHA, ALU.mult, W=[xtbs[kc]])
                        kb.tt(e, xt[:, kc, :W_], xt[:, kc, :W_], yv, ALU.add, R=[Ybs[kc]], W=[xtbs[kc]])
                    ln_stats(kb, C, xt, xtbs, KC, W_, sq, sqbs, ps1, ps1b, ps2, ps2b, mean, meanb, rstd, rstdb, nmr, nmrb, D, 0)
                    ln_apply(kb, xt, xtbs, KC, W_, rstd, rstdb, nmr, nmrb, lambda kc: xt[:, kc, :W_], xtbs,
                             lambda kc: lnv[:, 2, kc:kc + 1], lambda kc: lnv[:, 3, kc:kc + 1], extraR=[lnvb])
                    kb.dma(xtv[:, :, t0 + o_:t0 + o_ + W_], xt[:, :, :W_], R=xtbs, WA=[zb(f"XT{b}")])
                    o_ += W_

def lay_pf(v, n):
    sh = v.shape[:-1]
    return np.ascontiguousarray(np.swapaxes(v.reshape(*sh, n, 128), -1, -2))


def host_consts():
    c = np.zeros((128, 5 * 128), np.float32)
    c[:, 0:128] = np.eye(128)
    c[:, 128:256] = np.eye(128)[::-1]
    c[:, 256:384] = 1.0
    P = np.zeros((128, 128), np.float32)
    for blk in range(2):
        for i in range(32):
            P[blk * 64 + i, blk * 64 + i + 32] = -1.0
            P[blk * 64 + i + 32, blk * 64 + i] = 1.0
    c[:, 384:512] = P.T
    c[:, 512] = LN_EPS
    c[:, 513] = GN_EPS
    c[:, 514] = 128 * RMS_EPS
    inv = (10000.0 ** (-np.arange(32, dtype=np.float32) / 32)).astype(np.float32)
    t = np.arange(S)
    row = (t // 64).astype(np.float32)
    col = (t % 64).astype(np.float32)
    ang = np.zeros((128, S), np.float32)
    for dd in range(128):
        blk = dd // 64
        f = dd % 32
        ang[dd] = (row if blk == 0 else col) * inv[f]
    cos = np.ones((128, T), np.float32)
    sin = np.zeros((128, T), np.float32)
    cos[:, CL:] = np.cos(ang)
    sin[:, CL:] = np.sin(ang)
    return c, cos, sin


def make_in_maps(inp, ncores=8, small=False):
    c, cos, sin = host_consts()
    f = lambda a: np.ascontiguousarray(np.asarray(a, dtype=np.float32))
    shared = {
        "w_mod": f(inp["w_mod"]), "b_mod": lay_pf(f(inp["b_mod"]), 96), "w_in": f(inp["w_in"]),
        "b_gate": lay_pf(f(inp["b_gate"]), 48),
        "qk_norm": np.ascontiguousarray(np.stack([f(inp["q_norm"]), f(inp["k_norm"])], -1)),
        "w_att_o": f(inp["w_att_o"]), "rwkv_mu": f(inp["rwkv_mu"]), "rwkv_w0": f(inp["rwkv_w0"]),
        "rwkv_w2": f(inp["rwkv_w2"]).reshape(DEPTH, 128, 1024), "rwkv_a0": f(inp["rwkv_a0"]),
        "rwkv_a2": f(inp["rwkv_a2"]).reshape(DEPTH, 128, 1024), "rwkv_g2": f(inp["rwkv_g2"]),
        "rwkv_vecs": np.ascontiguousarray(np.stack([f(inp["rwkv_k_k"]), f(inp["rwkv_k_a"]), f(inp["rwkv_r_k"]).reshape(DEPTH, 1024),
                                                    f(inp["rwkv_ln_g"]), f(inp["rwkv_ln_b"])], 1)),
        "w_rwkv_o": f(inp["w_rwkv_o"]),
        "conv_w": np.ascontiguousarray(np.transpose(f(inp["conv_w"]).reshape(DEPTH, 31, 8, 128), (0, 3, 2, 1))),
        "conv_vecs": np.ascontiguousarray(np.stack([lay_pf(f(inp["conv_b"]), 8), lay_pf(f(inp["conv_ln_g"]), 8), lay_pf(f(inp["conv_ln_b"]), 8)], 2)),
        "w_conv_o": f(inp["w_conv_o"]), "w_out": f(inp["w_out"]),
        "ln_vecs": np.ascontiguousarray(np.stack([lay_pf(f(inp[k]), KC) for k in ("ln1_g", "ln1_b", "ln2_g", "ln2_b")], 2)),
        "w_gr": np.ascontiguousarray(np.concatenate([f(inp["w_group"]), f(inp["w_router"])], -1)),
        "b_gr": np.ascontiguousarray(np.concatenate([f(inp["b_group"]), f(inp["b_router"])], -1)),
        "w_e_gate": f(inp["w_e_gate"][:, :1] if small else inp["w_e_gate"]), "w_e_up": f(inp["w_e_up"][:, :1] if small else inp["w_e_up"]),
        "w_e_down": f(inp["w_e_down"][:, :1] if small else inp["w_e_down"]),
        "consts": c, "cos": cos, "sin": sin,
    }
    x = f(inp["x"]); ctx = f(inp["ctx"]); cc = f(inp["c"]); c_ctx = f(inp["c_ctx"])
    maps = []
    for i in range(ncores):
        rows = np.stack([cc[2 * i], cc[2 * i + 1], c_ctx], 0)
        cT = np.ascontiguousarray(np.transpose(rows.reshape(3, KC, 128), (2, 1, 0))).reshape(128, KC * 3)
        m = dict(shared)
        m["x"] = np.ascontiguousarray(x[2 * i:2 * i + 2])
        m["ctx"] = np.ascontiguousarray(ctx[2 * i:2 * i + 2])
        m["cT"] = cT
        maps.append(m)
    return maps


def kernel(**inputs):
    nc = build()
    maps = make_in_maps(inputs)
    res = run_bass_kernel_spmd(nc, maps, core_ids=list(range(8)))
    return np.concatenate([np.asarray(r["out"], dtype=np.float32) for r in res.results], axis=0)
```

```python
import numpy as np
from contextlib import ExitStack, contextmanager
import concourse.bass as bass
import concourse.mybir as mybir
from concourse.bass_utils import run_bass_kernel_spmd

F32 = mybir.dt.float32
BF16 = mybir.dt.bfloat16
ALU = mybir.AluOpType
AF = mybir.ActivationFunctionType
AX = mybir.AxisListType

NDS = 24
D = 2048
KC = 16
S = 2048
CL = 256
T = S + CL
NBL = 2
DEPTH = 2
RIN = 3456
INW = 13184
Q0, K0, V0, R0, C0, G0 = 0, 1024, 1280, 1536, 4992, 7040
NE = 32
FF = 1024
DN_ALPHA = float((2 * DEPTH) ** 0.25)
LN_EPS = 1e-6
RMS_EPS = 1e-6
GN_EPS = 64e-5
ATT_SCALE = 128 ** -0.5
NTB = T // 128
TILES = [(0, 256), (256, 512), (768, 512), (1280, 512), (1792, 512)]
TILES_LAT = TILES[1:]


class Buf:
    __slots__ = ("name", "w", "r")

    def __init__(self, name=""):
        self.name = name
        self.w = {}
        self.r = {}


def _merge(dst, src):
    for k, (sem, val) in src.items():
        if k not in dst or dst[k][1] < val:
            dst[k] = (sem, val)


class KB:
    def __init__(self, nc):
        self.nc = nc
        self.es = ExitStack()
        self.eng = {"pe": nc.tensor, "act": nc.scalar, "dve": nc.vector, "pool": nc.gpsimd, "sp": nc.sync}
        self.sem = {k: self.es.enter_context(nc.semaphore("s_" + k)) for k in self.eng}
        self.cnt = {k: 0 for k in self.eng}
        self.waited = {k: {} for k in self.eng}
        self.dsems = [self.es.enter_context(nc.semaphore(f"dq{i}")) for i in range(NDS)]
        self.dval = [0] * NDS
        self.dnext = 0
        self.nbuf = 0
        self.ninstr = 0

    def buf(self, name=""):
        self.nbuf += 1
        return Buf(name)

    def _wait(self, e, toks):
        for key, (sem, val) in toks.items():
            if key == e and e in ("pe", "sp"):
                continue
            if self.waited[e].get(key, 0) < val:
                self.eng[e].wait_ge(sem, val)
                self.waited[e][key] = val

    def _deps(self, reads, writes, wacc=()):
        toks = {}
        for b in reads:
            _merge(toks, b.w)
        for b in writes:
            _merge(toks, b.w)
            _merge(toks, b.r)
        for b in wacc:
            _merge(toks, b.r)
        return toks

    def _post(self, key, tok, reads, writes, wacc=()):
        for b in writes:
            b.w = {key: tok}
            b.r = {}
        for b in wacc:
            b.w[key] = tok
        for b in reads:
            if b not in writes:
                b.r[key] = tok

    def op(self, e, fn, R=(), W=(), WA=()):
        self._wait(e, self._deps(R, W, WA))
        ins = fn(self.eng[e])
        self.cnt[e] += 1
        self.ninstr += 1
        ins.then_inc(self.sem[e], 1)
        self._post(e, (self.sem[e], self.cnt[e]), R, W, WA)
        return ins

    def mm(self, fns, R=(), W=()):
        e = "pe"
        self._wait(e, self._deps(R, W))
        ins = None
        for fn in fns:
            ins = fn(self.eng[e])
            self.ninstr += 1
        self.cnt[e] += 1
        ins.then_inc(self.sem[e], 1)
        self._post(e, (self.sem[e], self.cnt[e]), R, W)

    def dma(self, out, in_, R=(), W=(), WA=(), q="sp"):
        i = self.dnext
        self.dnext = (i + 1) % NDS
        toks = self._deps(R, W, WA)
        key = ("d", i)
        if self.dval[i]:
            _merge(toks, {key: (self.dsems[i], self.dval[i])})
        self._wait(q, toks)
        ins = self.eng[q].dma_start(out=out, in_=in_)
        self.ninstr += 1
        self.dval[i] += 16
        ins.then_inc(self.dsems[i], 16)
        self._post(key, (self.dsems[i], self.dval[i]), R, W, WA)

    def all_tokens(self):
        toks = {}
        for e in self.eng:
            if self.cnt[e]:
                toks[e] = (self.sem[e], self.cnt[e])
        for i in range(NDS):
            if self.dval[i]:
                toks[("d", i)] = (self.dsems[i], self.dval[i])
        return toks

    def barrier(self):
        toks = self.all_tokens()
        for e in self.eng:
            self._wait(e, dict(toks))

    def finish(self):
        self._wait("sp", self.all_tokens())

    @contextmanager
    def scope(self):
        es = ExitStack()
        try:
            yield Scope(self, es)
        finally:
            self.barrier()
            es.close()

    def tt(self, e, out, in0, in1, op, R=(), W=()):
        return self.op(e, lambda g: g.tensor_tensor(out=out, in0=in0, in1=in1, op=op), R, W)

    def ts(self, e, out, in0, s1, op0, s2=None, op1=None, R=(), W=()):
        if op1 is None:
            return self.op(e, lambda g: g.tensor_scalar(out=out, in0=in0, scalar1=s1, scalar2=None, op0=op0), R, W)
        return self.op(e, lambda g: g.tensor_scalar(out=out, in0=in0, scalar1=s1, scalar2=s2, op0=op0, op1=op1), R, W)

    def stt(self, e, out, in0, scalar, in1, op0, op1, R=(), W=()):
        return self.op(e, lambda g: g.scalar_tensor_tensor(out=out, in0=in0, scalar=scalar, in1=in1, op0=op0, op1=op1), R, W)

    def act(self, out, in_, func, R=(), W=(), **kw):
        return self.op("act", lambda g: g.activation(out=out, in_=in_, func=func, **kw), R, W)

    def cp(self, e, out, in_, R=(), W=()):
        if e == "act":
            return self.op(e, lambda g: g.copy(out=out, in_=in_), R, W)
        return self.op(e, lambda g: g.tensor_copy(out=out, in_=in_), R, W)

    def red(self, out, in_, op, R=(), W=()):
        return self.op("dve", lambda g: g.tensor_reduce(out=out, in_=in_, axis=AX.X, op=op), R, W)


class Scope:
    def __init__(self, kb, es):
        self.kb = kb
        self.es = es

    def sb(self, shape, dtype, name="t", nb=1):
        self.kb.nbuf += 1
        t = self.es.enter_context(self.kb.nc.sbuf_tensor(f"{name}_{self.kb.nbuf}", list(shape), dtype))
        if nb == 1:
            return t, Buf(name)
        return t, [Buf(name) for _ in range(nb)]

    def ps(self, shape=(128, 512), dtype=F32, name="p"):
        self.kb.nbuf += 1
        t = self.es.enter_context(self.kb.nc.psum_tensor(f"{name}_{self.kb.nbuf}", list(shape), dtype))
        return t, Buf(name)


class Ctx:
    pass


def pm(ap, p=128):
    return ap.rearrange("(kc p) n -> p kc n", p=p)


def build(nlayers=DEPTH, dbg=(), small=False):
    nc = bass.Bass("TRN2", target_bir_lowering=False)
    es = ExitStack()
    es.enter_context(nc.allow_non_contiguous_dma("small param layouts"))
    es.enter_context(nc.allow_low_precision("bf16 matmul operands, fp32 accumulation"))

    def din(name, shape, dt=F32):
        return nc.dram_tensor(name, list(shape), dt, kind="ExternalInput").ap()

    def dscr(name, shape, dt=F32):
        kind = "ExternalOutput" if name in dbg else "Internal"
        return nc.dram_tensor(name, list(shape), dt, kind=kind).ap()

    I = Ctx()
    I.x = din("x", [NBL, S, D])
    I.ctx = din("ctx", [NBL, CL, D])
    I.cT = din("cT", [128, KC * 3])
    I.w_mod = din("w_mod", [DEPTH, D, 6 * D])
    I.b_mod = din("b_mod", [DEPTH, 128, 96])
    I.w_in = din("w_in", [DEPTH, D, INW])
    I.b_gate = din("b_gate", [DEPTH, 128, 48])
    I.qk_norm = din("qk_norm", [DEPTH, 128, 2])
    I.w_att_o = din("w_att_o", [DEPTH, 1024, D])
    I.rwkv_mu = din("rwkv_mu", [DEPTH, 2, RIN])
    I.rwkv_w0 = din("rwkv_w0", [DEPTH, 2, 1024])
    I.rwkv_w2 = din("rwkv_w2", [DEPTH, 128, 1024])
    I.rwkv_a0 = din("rwkv_a0", [DEPTH, 2, 1024])
    I.rwkv_a2 = din("rwkv_a2", [DEPTH, 128, 1024])
    I.rwkv_g2 = din("rwkv_g2", [DEPTH, 128, 1024])
    I.rwkv_vecs = din("rwkv_vecs", [DEPTH, 5, 1024])
    I.w_rwkv_o = din("w_rwkv_o", [DEPTH, 1024, D])
    I.conv_w = din("conv_w", [DEPTH, 128, 8, 31])
    I.conv_vecs = din("conv_vecs", [DEPTH, 128, 3, 8])
    I.w_conv_o = din("w_conv_o", [DEPTH, 1024, D])
    I.w_out = din("w_out", [DEPTH, D, D])
    I.ln_vecs = din("ln_vecs", [DEPTH, 128, 4, KC])
    I.w_gr = din("w_gr", [DEPTH, D, 36])
    I.b_gr = din("b_gr", [DEPTH, 36])
    ned = 1 if small else NE
    I.w_e_gate = din("w_e_gate", [DEPTH, ned, D, FF])
    I.w_e_up = din("w_e_up", [DEPTH, ned, D, FF])
    I.w_e_down = din("w_e_down", [DEPTH, ned, FF, D])
    I.consts = din("consts", [128, 5 * 128])
    I.cos = din("cos", [128, T])
    I.sin = din("sin", [128, T])
    out = nc.dram_tensor("out", [NBL, S, D], F32, kind="ExternalOutput").ap()

    Z = Ctx()
    Z.XT = [dscr(f"XT{b}", [D, T]) for b in range(NBL)]
    Z.RT = [dscr(f"RT{b}", [D, T]) for b in range(NBL)]
    Z.QT = [dscr(f"QT{b}", [1024, T], BF16) for b in range(NBL)]
    Z.ZR = [dscr(f"ZR{b}", [T, RIN]) for b in range(NBL)]
    Z.CU = [dscr(f"CU{b}", [1024, T]) for b in range(NBL)]
    Z.G = [dscr(f"G{b}", [3 * D, T], BF16) for b in range(NBL)]
    Z.ATT = [dscr(f"ATT{b}", [1024, T], BF16) for b in range(NBL)]
    Z.RWO = [dscr(f"RWO{b}", [1024, T], BF16) for b in range(NBL)]
    Z.CVO = [dscr(f"CVO{b}", [1024, T], BF16) for b in range(NBL)]
    Z.MT = [dscr(f"MT{b}", [D, T], BF16) for b in range(NBL)]
    Z.H2 = [dscr(f"H2{b}", [D, T], BF16) for b in range(NBL)]
    Z.WTT = dscr("WTT", [NE, NBL * T])
    Z.SC = dscr("SC", [2 * NBL * 16, T, 384])
    Z.YS = dscr("YS", [2 * NBL * 16, T, 64])
    Z.AUX = [dscr(f"AUX{b}", [T, 2, 1024]) for b in range(NBL)]
    Z.RK = [dscr(f"RK{b}", [2, T, 16]) for b in range(NBL)]
    Z.bufs = {}
    if "HTD0" in dbg:
        Z.HTD = [dscr(f"HTD{b}", [D, T]) for b in range(NBL)]
    if "MODD" in dbg:
        Z.MODD = dscr("MODD", [128, DEPTH * 96 * 3])

    def zb(ap_name):
        if ap_name not in Z.bufs:
            Z.bufs[ap_name] = Buf(ap_name)
        return Z.bufs[ap_name]

    kb = KB(nc)
    C = Ctx()
    top = ExitStack()
    tsc = Scope(kb, top)
    cst, cstb = tsc.sb([128, 5 * 128], F32, "consts")
    kb.dma(cst[:], I.consts[:, :], W=[cstb])
    C.cst, C.cstb = cst, cstb
    C.ident, C.J, C.ones_f, C.PT = (cst[:, i_ * 128:(i_ + 1) * 128] for i_ in range(4))
    ones_b, ones_bb = tsc.sb([128, 128], BF16, "ones_b")
    kb.cp("dve", ones_b[:], cst[:, 256:384], R=[cstb], W=[ones_bb])
    C.ones_b, C.ones_bb = ones_b, ones_bb
    MOD, MODb = tsc.sb([128, DEPTH, 96, 3], F32, "MOD")
    C.MOD, C.MODb = MOD, MODb
    stop = [d_ for d_ in dbg if d_.startswith("stop")]
    stop = stop[0] if stop else ""

    phase0(kb, C, I, Z, zb)
    phaseA(kb, C, I, nlayers)
    if hasattr(Z, "MODD"):
        kb.dma(Z.MODD[:, :], C.MOD[:].rearrange("p l j r -> p (l j r)"), R=[C.MODb])
    for l in range(nlayers):
        last = (l == DEPTH - 1)
        for b in range(NBL):
            phaseB(kb, C, I, Z, zb, l, b, last)
        if stop == f"stopB{l}":
            break
        phaseD(kb, C, I, Z, zb, l)
        if stop == f"stopD{l}":
            break
        phaseE(kb, C, I, Z, zb, l)
        if stop == f"stopE{l}":
            break
        for b in range(NBL):
            phaseF(kb, C, I, Z, zb, l, b, last)
        if stop == f"stopF{l}":
            break
        for b in range(NBL):
            phaseG(kb, C, I, Z, zb, l, b, last)
        if stop == f"stopG{l}":
            break
        for b in range(NBL):
            phaseH(kb, C, I, Z, zb, l, b, last)
            phaseI(kb, C, I, Z, zb, l, b, last)
            phaseJ(kb, C, I, Z, zb, l, b, last)
        if stop == f"stopJ{l}":
            break
        phaseK(kb, C, I, Z, zb, l, last)
    phaseFinal(kb, C, I, Z, zb, out)
    kb.finish()
    top.close()
    print("instructions:", kb.ninstr, flush=True)
    return nc


def ln_stats(kb, C, x, xbs, nk, W, sq, sqbs, ps1, ps1b, ps2, ps2b, mean, meanb, rstd, rstdb, nmr, nmrb, dn, epscol):
    for kc in range(nk):
        if kc % 2 == 0:
            kb.act(sq[:, kc, :W], x[:, kc, :W], AF.Square, R=[xbs[kc]], W=[sqbs[kc]])
        else:
            kb.tt("pool", sq[:, kc, :W], x[:, kc, :W], x[:, kc, :W], ALU.mult, R=[xbs[kc]], W=[sqbs[kc]])
    kb.mm([lambda e, kc=kc: e.matmul(ps1[:, :W], lhsT=C.ones_f, rhs=x[:, kc, :W], start=(kc == 0), stop=(kc == nk - 1))
           for kc in range(nk)], R=list(xbs[:nk]) + [C.cstb], W=[ps1b])
    kb.mm([lambda e, kc=kc: e.matmul(ps2[:, :W], lhsT=C.ones_f, rhs=sq[:, kc, :W], start=(kc == 0), stop=(kc == nk - 1))
           for kc in range(nk)], R=list(sqbs[:nk]) + [C.cstb], W=[ps2b])
    kb.act(mean[:, :W], ps1[:, :W], AF.Copy, R=[ps1b], W=[meanb], scale=1.0 / dn)
    kb.tt("dve", rstd[:, :W], mean[:, :W], mean[:, :W], ALU.mult, R=[meanb], W=[rstdb])
    kb.stt("dve", rstd[:, :W], ps2[:, :W], 1.0 / dn, rstd[:, :W], ALU.mult, ALU.subtract, R=[ps2b, rstdb], W=[rstdb])
    kb.ts("dve", rstd[:, :W], rstd[:, :W], 0.0, ALU.max, R=[rstdb], W=[rstdb])
    kb.act(rstd[:, :W], rstd[:, :W], AF.Sqrt, R=[rstdb, C.cstb], W=[rstdb], bias=C.cst[:, 512 + epscol:513 + epscol], scale=1.0)
    kb.op("dve", lambda g: g.reciprocal(out=rstd[:, :W], in_=rstd[:, :W]), R=[rstdb], W=[rstdb])
    kb.stt("dve", nmr[:, :W], mean[:, :W], -1.0, rstd[:, :W], ALU.mult, ALU.mult, R=[meanb, rstdb], W=[nmrb])


def ln_apply(kb, x, xbs, nk, W, rstd, rstdb, nmr, nmrb, out_of, outbs, a_of, b_of, extraR=()):
    for kc in range(nk):
        e = "dve" if kc % 2 == 0 else "pool"
        t = x[:, kc, :W]
        kb.tt(e, t, t, rstd[:, :W], ALU.mult, R=[rstdb], W=[xbs[kc]])
        kb.tt(e, t, t, nmr[:, :W], ALU.add, R=[nmrb], W=[xbs[kc]])
        kb.act(out_of(kc), t, AF.Identity, R=[xbs[kc]] + list(extraR), W=[outbs[kc]], scale=a_of(kc), bias=b_of(kc))


def phase0(kb, C, I, Z, zb):
    with kb.scope() as sc:
        xin = [sc.sb([128, D], F32, "xin") for _ in range(2)]
        xo = [sc.sb([128, KC, 128], F32, "xo", nb=4) for _ in range(2)]
        pst = [sc.ps() for _ in range(4)]
        n = 0
        for b in range(NBL):
            xtv = pm(Z.XT[b])
            for tb in range(NTB):
                src = I.ctx[b, tb * 128:(tb + 1) * 128, :] if tb < 2 else I.x[b, (tb - 2) * 128:(tb - 1) * 128, :]
                xi, xib = xin[n % 2]
                o, obs = xo[n % 2]
                n += 1
                kb.dma(xi[:], src, W=[xib])
                for g in range(4):
                    p, pb = pst[g]
                    kb.mm([lambda e, j=j, g=g, p=p, xi=xi: e.transpose(p[:, j * 128:(j + 1) * 128], xi[:, (g * 4 + j) * 128:(g * 4 + j + 1) * 128], C.ident)
                           for j in range(4)], R=[xib, C.cstb], W=[pb])
                    kb.cp("act" if g % 2 == 0 else "dve", o[:, g * 4:(g + 1) * 4, :], p[:, :].rearrange("p (a t) -> p a t", t=128), R=[pb], W=[obs[g]])
                kb.dma(xtv[:, :, tb * 128:(tb + 1) * 128], o[:], R=obs, WA=[zb(f"XT{b}")])


def phaseFinal(kb, C, I, Z, zb, out):
    with kb.scope() as sc:
        xin = [sc.sb([128, KC, 128], F32, "fin") for _ in range(2)]
        xo = [sc.sb([128, D], F32, "fo", nb=4) for _ in range(2)]
        pst = [sc.ps() for _ in range(4)]
        n = 0
        for b in range(NBL):
            xtv = pm(Z.XT[b])
            for tb in range(2, NTB):
                xi, xib = xin[n % 2]
                o, obs = xo[n % 2]
                n += 1
                kb.dma(xi[:], xtv[:, :, tb * 128:(tb + 1) * 128], R=[zb(f"XT{b}")], W=[xib])
                for g in range(4):
                    p, pb = pst[g]
                    kb.mm([lambda e, j=j, g=g, p=p, xi=xi: e.transpose(p[:, j * 128:(j + 1) * 128], xi[:, g * 4 + j, :], C.ident)
                           for j in range(4)], R=[xib, C.cstb], W=[pb])
                    kb.cp("act" if g % 2 == 0 else "dve", o[:, g * 512:(g + 1) * 512], p[:, :], R=[pb], W=[obs[g]])
                kb.dma(out[b, (tb - 2) * 128:(tb - 1) * 128, :], o[:], R=obs)


def phaseA(kb, C, I, nlayers):
    with kb.scope() as sc:
        cT, cTb = sc.sb([128, KC, 3], F32, "cT")
        kb.dma(cT[:], I.cT.rearrange("p (k r) -> p k r", r=3), W=[cTb])
        sg, sgb = sc.sb([128, KC, 3], F32, "sg")
        kb.act(sg[:], cT[:], AF.Silu, R=[cTb], W=[sgb])
        wts = [sc.sb([128, KC, 512], F32, "wmod") for _ in range(2)]
        bm, bmb = sc.sb([128, 96], F32, "bm")
        ps, psb = sc.ps()
        for l in range(nlayers):
            kb.dma(bm[:], I.b_mod[l], W=[bmb])
            for g in range(24):
                w, wb = wts[g % 2]
                kb.dma(w[:], pm(I.w_mod[l, :, g * 512:(g + 1) * 512]), W=[wb])
                for j in range(4):
                    mc = g * 4 + j
                    kb.mm([lambda e, kc=kc, j=j, w=w, mc=mc: e.matmul(ps[:, mc * 3:(mc + 1) * 3], lhsT=w[:, kc, j * 128:(j + 1) * 128], rhs=sg[:, kc, :],
                                                                   start=(kc == 0), stop=(kc == KC - 1)) for kc in range(KC)], R=[wb, sgb], W=[psb])
            kb.tt("dve", C.MOD[:, l, :, :], ps[:, 0:288].rearrange("p (j r) -> p j r", r=3), bm[:].unsqueeze(2).to_broadcast([128, 96, 3]), ALU.add,
                  R=[psb, bmb], W=[C.MODb])
            for (a, b_) in ((16, 32), (64, 80)):
                kb.ts("dve", C.MOD[:, l, a:b_, :], C.MOD[:, l, a:b_, :], 1.0, ALU.add, R=[C.MODb], W=[C.MODb])


def win_groups():
    gs = [(0, 512, "q", 0), (512, 512, "q", 4), (K0, 256, "k", 0), (V0, 256, "v", 0)]
    for i in range(6):
        gs.append((R0 + i * 512, 512, "r", i * 512))
    gs.append((R0 + 3072, 384, "r", 3072))
    for i in range(4):
        gs.append((C0 + i * 256, 512, "c", i))
    for i in range(12):
        gs.append((G0 + i * 512, 512, "g", i))
    return gs


def phaseB(kb, C, I, Z, zb, l, b, last):
    tiles = TILES
    with kb.scope() as scB:
        KT, KTb = scB.sb([128, 2, T], BF16, "KT")
        VT, VTb = scB.sb([128, NTB, 256], BF16, "VT")
        with kb.scope() as scH:
            HT, HTbs = scH.sb([128, KC, T], BF16, "HT", nb=KC)
            with kb.scope() as sc:
                xs = [sc.sb([128, KC, 256], F32, "xs", nb=KC) for _ in range(2)]
                sq, sqbs = sc.sb([128, KC, 256], F32, "sq", nb=KC)
                ps1, ps1b = sc.ps()
                ps2, ps2b = sc.ps()
                mean, meanb = sc.sb([128, 256], F32, "mean")
                rstd, rstdb = sc.sb([128, 256], F32, "rstd")
                nmr, nmrb = sc.sb([128, 256], F32, "nmr")
                xtv = pm(Z.XT[b])
                for ti in range(T // 256):
                    t0 = ti * 256
                    x, xbs = xs[ti % 2]
                    r = 2 if t0 < CL else b
                    kb.dma(x[:], xtv[:, :, t0:t0 + 256], R=[zb(f"XT{b}")], W=xbs)
                    ln_stats(kb, C, x, xbs, KC, 256, sq, sqbs, ps1, ps1b, ps2, ps2b, mean, meanb, rstd, rstdb, nmr, nmrb, D, 0)
                    ln_apply(kb, x, xbs, KC, 256, rstd, rstdb, nmr, nmrb, lambda kc: HT[:, kc, t0:t0 + 256], HTbs,
                             lambda kc: C.MOD[:, l, 16 + kc, r:r + 1], lambda kc: C.MOD[:, l, kc, r:r + 1], extraR=[C.MODb])
            if hasattr(Z, "HTD"):
                with kb.scope() as sc:
                    st, stb = sc.sb([128, KC, 256], F32, "st")
                    for ti in range(T // 256):
                        kb.cp("dve", st[:], HT[:, :, ti * 256:(ti + 1) * 256], R=HTbs, W=[stb])
                        kb.dma(pm(Z.HTD[b])[:, :, ti * 256:(ti + 1) * 256], st[:], R=[stb])
            with kb.scope() as sc:
                cos, cosb = sc.sb([128, T], F32, "cos")
                sin, sinb = sc.sb([128, T], F32, "sin")
                kb.dma(cos[:], I.cos[:, :], W=[cosb])
                kb.dma(sin[:], I.sin[:, :], W=[sinb])
                qkn, qknb = sc.sb([128, 2], F32, "qkn")
                kb.dma(qkn[:], I.qk_norm[l], W=[qknb])
                kb.ts("dve", qkn[:], qkn[:], float(128 ** 0.5), ALU.mult, R=[qknb], W=[qknb])
                bg, bgb = sc.sb([128, 48], F32, "bg")
                kb.dma(bg[:], I.b_gate[l], W=[bgb])
                wts = [sc.sb([128, KC, 512], BF16, "win") for _ in range(2)]
                psM = [sc.ps() for _ in range(4)]
                psA, psAb = sc.ps()
                psB, psBb = sc.ps()
                sq16 = [sc.sb([128, 512], BF16, "sq16") for _ in range(2)]
                r1s = [sc.sb([128, 512], F32, "r1") for _ in range(2)]
                qns = [sc.sb([128, 512], F32, "qn") for _ in range(2)]
                t1s = [sc.sb([128, 512], F32, "t1") for _ in range(2)]
                st16 = [sc.sb([128, 512], BF16, "st16") for _ in range(3)]
                st32 = [sc.sb([128, 512], F32, "st32") for _ in range(3)]
                cnt = {"m": 0, "e": 0, "s16": 0, "s32": 0}

                def gemm_fm(w, wb, j, t0, W_):
                    p, pb = psM[cnt["m"] % 4]
                    cnt["m"] += 1
                    kb.mm([lambda e, kc=kc: e.matmul(p[:, :W_], lhsT=w[:, kc, j * 128:(j + 1) * 128], rhs=HT[:, kc, t0:t0 + W_],
                                                     start=(kc == 0), stop=(kc == KC - 1)) for kc in range(KC)], R=[wb] + HTbs, W=[pb])
                    return p, pb

                def gemm_tm(w, wb, tb, n):
                    p, pb = psM[cnt["m"] % 4]
                    cnt["m"] += 1
                    kb.mm([lambda e, kc=kc: e.matmul(p[:, :n], lhsT=HT[:, kc, tb * 128:(tb + 1) * 128], rhs=w[:, kc, 0:n],
                                                     start=(kc == 0), stop=(kc == KC - 1)) for kc in range(KC)], R=[wb] + HTbs, W=[pb])
                    return p, pb

                for gi, (c0, n, kind, aux) in enumerate(win_groups()):
                    w, wb = wts[gi % 2]
                    if kind == "c":
                        kb.dma(w[:, :, 0:256], pm(I.w_in[l, :, c0:c0 + 256]), W=[wb], q="pool")
                        kb.dma(w[:, :, 256:512], pm(I.w_in[l, :, c0 + 1024:c0 + 1280]), WA=[wb], q="pool")
                    else:
                        kb.dma(w[:, :, 0:n], pm(I.w_in[l, :, c0:c0 + n]), W=[wb], q="pool")
                    if kind in ("q", "k"):
                        for j in range(n // 128):
                            hd = aux + j
                            for (t0, W_) in tiles:
                                if kind == "q" and last and t0 < CL:
                                    continue
                                p, pb = gemm_fm(w, wb, j, t0, W_)
                                i2 = cnt["e"] % 2
                                cnt["e"] += 1
                                s16, s16b = sq16[i2]
                                r1, r1b = r1s[i2]
                                qn, qnb = qns[i2]
                                t1, t1b = t1s[i2]
                                gc = 0 if kind == "q" else 1
                                kb.act(s16[:, :W_], p[:, :W_], AF.Square, R=[pb], W=[s16b])
                                kb.mm([lambda e: e.matmul(psA[:, :W_], lhsT=C.ones_b[:], rhs=s16[:, :W_], start=True, stop=True)], R=[s16b, C.ones_bb], W=[psAb])
                                kb.act(r1[:, :W_], psA[:, :W_], AF.Sqrt, R=[psAb, C.cstb], W=[r1b], bias=C.cst[:, 514:515], scale=1.0)
                                kb.op("dve", lambda g: g.reciprocal(out=r1[:, :W_], in_=r1[:, :W_]), R=[r1b], W=[r1b])
                                kb.stt("dve", qn[:, :W_], p[:, :W_], qkn[:, gc:gc + 1], r1[:, :W_], ALU.mult, ALU.mult, R=[pb, qknb, r1b], W=[qnb])
                                kb.mm([lambda e: e.matmul(psB[:, :W_], lhsT=C.PT, rhs=qn[:, :W_], start=True, stop=True)], R=[qnb, C.cstb], W=[psBb])
                                kb.tt("pool", t1[:, :W_], qn[:, :W_], cos[:, t0:t0 + W_], ALU.mult, R=[qnb, cosb], W=[t1b])
                                kb.tt("dve", qn[:, :W_], psB[:, :W_], sin[:, t0:t0 + W_], ALU.mult, R=[psBb, sinb], W=[qnb])
                                if kind == "k":
                                    kb.tt("dve", KT[:, hd, t0:t0 + W_], t1[:, :W_], qn[:, :W_], ALU.add, R=[t1b, qnb], W=[KTb])
                                else:
                                    o16, o16b = st16[cnt["s16"] % 3]
                                    cnt["s16"] += 1
                                    kb.tt("dve", o16[:, :W_], t1[:, :W_], qn[:, :W_], ALU.add, R=[t1b, qnb], W=[o16b])
                                    kb.dma(Z.QT[b][hd * 128:(hd + 1) * 128, t0:t0 + W_], o16[:, :W_], R=[o16b], WA=[zb(f"QT{b}")])
                    elif kind == "v":
                        for tb in range(NTB):
                            p, pb = gemm_tm(w, wb, tb, 256)
                            kb.cp("act", VT[:, tb, :], p[:, :256], R=[pb], W=[VTb])
                    elif kind == "r":
                        for tb in range(NTB):
                            p, pb = gemm_tm(w, wb, tb, n)
                            o32, o32b = st32[cnt["s32"] % 3]
                            cnt["s32"] += 1
                            kb.cp("act" if tb % 2 == 0 else "dve", o32[:, :n], p[:, :n], R=[pb], W=[o32b])
                            kb.dma(Z.ZR[b][tb * 128:(tb + 1) * 128, aux:aux + n], o32[:, :n], R=[o32b], WA=[zb(f"ZR{b}")])
                    elif kind == "c":
                        for jj in range(2):
                            cc = aux * 2 + jj
                            for (t0, W_) in tiles:
                                if last and t0 < CL:
                                    continue
                                pv, pvb = gemm_fm(w, wb, jj, t0, W_)
                                pg, pgb = gemm_fm(w, wb, 2 + jj, t0, W_)
                                sgt, sgtb = st32[cnt["s32"] % 3]
                                cnt["s32"] += 1
                                kb.act(sgt[:, :W_], pg[:, :W_], AF.Sigmoid, R=[pgb], W=[sgtb])
                                kb.tt("dve", sgt[:, :W_], pv[:, :W_], sgt[:, :W_], ALU.mult, R=[pvb, sgtb], W=[sgtb])
                                kb.dma(Z.CU[b][cc * 128:(cc + 1) * 128, t0:t0 + W_], sgt[:, :W_], R=[sgtb], WA=[zb(f"CU{b}")])
                    elif kind == "g":
                        for j in range(4):
                            mc = aux * 4 + j
                            for (t0, W_) in tiles:
                                if last and t0 < CL:
                                    continue
                                p, pb = gemm_fm(w, wb, j, t0, W_)
                                o16, o16b = st16[cnt["s16"] % 3]
                                cnt["s16"] += 1
                                kb.act(o16[:, :W_], p[:, :W_], AF.Sigmoid, R=[pb, bgb], W=[o16b], bias=bg[:, mc:mc + 1], scale=1.0)
                                kb.dma(Z.G[b][mc * 128:(mc + 1) * 128, t0:t0 + W_], o16[:, :W_], R=[o16b], WA=[zb(f"G{b}")])
        with kb.scope() as sc:
            qts = [sc.sb([128, 512], BF16, "qt") for _ in range(2)]
            pTs = [sc.sb([128, 512], BF16, "pT") for _ in range(3)]
            psS = [sc.ps() for _ in range(3)]
            psO = [sc.ps() for _ in range(2)]
            psL = [sc.ps() for _ in range(2)]
            rls = [sc.sb([128, 512], F32, "rl") for _ in range(2)]
            ats = [sc.sb([128, 512], BF16, "at") for _ in range(2)]
            its = [(h, t0, W_) for h in range(8) for (t0, W_) in tiles if not (t0 < CL and last)]

            def load_q(n_):
                h_, t0_, W__ = its[n_]
                qt_, qtb_ = qts[n_ % 2]
                kb.dma(qt_[:, :W__], Z.QT[b][h_ * 128:(h_ + 1) * 128, t0_:t0_ + W__], R=[zb(f"QT{b}")], W=[qtb_])

            load_q(0)
            m = 0
            for n, (h, t0, W_) in enumerate(its):
                g = h // 4
                kbl = [0, 1] if t0 < CL else list(range(NTB))
                qt, qtb = qts[n % 2]
                po, pob = psO[n % 2]
                pl, plb = psL[n % 2]
                rl, rlb = rls[n % 2]
                at, atb = ats[n % 2]
                if n + 1 < len(its):
                    load_q(n + 1)

                def s_mm(ki_):
                    kblk_ = kbl[ki_]
                    pS_, pSb_ = psS[(m + ki_) % 3]
                    kb.mm([lambda e: e.matmul(pS_[:, :W_], lhsT=KT[:, g, kblk_ * 128:(kblk_ + 1) * 128], rhs=qt[:, :W_], start=True, stop=True)],
                          R=[KTb, qtb], W=[pSb_])

                s_mm(0)
                for ki, kblk in enumerate(kbl):
                    pS, pSb = psS[(m + ki) % 3]
                    pT, pTb = pTs[(m + ki) % 3]
                    if ki + 1 < len(kbl):
                        s_mm(ki + 1)
                    kb.act(pT[:, :W_], pS[:, :W_], AF.Exp, R=[pSb], W=[pTb], scale=float(ATT_SCALE))
                    kb.mm([lambda e: e.matmul(po[:, :W_], lhsT=VT[:, kblk, g * 128:(g + 1) * 128], rhs=pT[:, :W_], start=(ki == 0), stop=(ki == len(kbl) - 1))],
                          R=[VTb, pTb], W=[pob])
                    kb.mm([lambda e: e.matmul(pl[:, :W_], lhsT=C.ones_b[:], rhs=pT[:, :W_], start=(ki == 0), stop=(ki == len(kbl) - 1))],
                          R=[C.ones_bb, pTb], W=[plb])
                m += len(kbl)
                kb.op("dve", lambda g_: g_.reciprocal(out=rl[:, :W_], in_=pl[:, :W_]), R=[plb], W=[rlb])
                kb.tt("dve", at[:, :W_], po[:, :W_], rl[:, :W_], ALU.mult, R=[pob, rlb], W=[atb])
                kb.dma(Z.ATT[b][h * 128:(h + 1) * 128, t0:t0 + W_], at[:, :W_], R=[atb], WA=[zb(f"ATT{b}")])

def sc_row(d, b):
    return (d * NBL + b) * 16


def rev_block(tb):
    return (1 - tb) if tb < 2 else (19 - tb)


def phaseD(kb, C, I, Z, zb, l):
    with kb.scope() as sc:
        bc = lambda ap: ap.partition_broadcast(128)
        MU = [sc.sb([128, RIN], F32, "mu") for _ in range(2)]
        W0 = [sc.sb([128, 1024], F32, "w0") for _ in range(2)]
        A0 = [sc.sb([128, 1024], F32, "a0") for _ in range(2)]
        for i in range(2):
            kb.dma(MU[i][0][:], bc(I.rwkv_mu[l, i, :]), W=[MU[i][1]])
            kb.dma(W0[i][0][:], bc(I.rwkv_w0[l, i, :]), W=[W0[i][1]])
            kb.dma(A0[i][0][:], bc(I.rwkv_a0[l, i, :]), W=[A0[i][1]])
        KK, KKb = sc.sb([128, 1024], F32, "kk")
        KA, KAb = sc.sb([128, 1024], F32, "ka")
        OM, OMb = sc.sb([128, 1024], F32, "om")
        RKt, RKtb = sc.sb([128, 1024], F32, "rkt")
        kb.dma(KK[:], bc(I.rwkv_vecs[l, 0, :]), W=[KKb])
        kb.dma(KA[:], bc(I.rwkv_vecs[l, 1, :]), W=[KAb])
        kb.dma(RKt[:], bc(I.rwkv_vecs[l, 2, :]), W=[RKtb])
        kb.ts("dve", OM[:], KA[:], -1.0, ALU.mult, 1.0, ALU.add, R=[KAb], W=[OMb])
        W2, W2b = sc.sb([128, 1024], BF16, "w2")
        A2, A2b = sc.sb([128, 1024], BF16, "a2")
        G2, G2b = sc.sb([128, 1024], BF16, "g2")
        kb.dma(W2[:], I.rwkv_w2[l], W=[W2b], q="pool")
        kb.dma(A2[:], I.rwkv_a2[l], W=[A2b], q="pool")
        kb.dma(G2[:], I.rwkv_g2[l], W=[G2b], q="pool")
        Zc, Zcb = sc.sb([128, RIN], F32, "zc")
        Zp, Zpb = sc.sb([128, RIN], F32, "zp")
        Zn, Znb = sc.sb([128, RIN], F32, "zn")
        Zr, Zrb = sc.sb([128, RIN], F32, "zr")
        X1, X1b = sc.sb([128, 1024], F32, "x1")
        DEC, DECb = sc.sb([128, 1024], F32, "dec")
        A_, A_b = sc.sb([128, 1024], F32, "a_")
        KKn, KKnb = sc.sb([128, 1024], F32, "kkn")
        Bt, Btb = sc.sb([128, 1024], F32, "bt")
        KD, KDb = sc.sb([128, 1024], F32, "kd")
        TM, TMb = sc.sb([128, 1024], F32, "tm")
        TG, TGb = sc.sb([128, 1024], F32, "tg")
        wlT, wlTb = sc.sb([128, 128], BF16, "wlT")
        alT, alTb = sc.sb([128, 128], BF16, "alT")
        glT, glTb = sc.sb([128, 128], BF16, "glT")
        ssq, ssqb = sc.sb([128, 16], F32, "ssq")
        rk, rkb = sc.sb([128, 16], F32, "rk")
        pJ = [sc.ps() for _ in range(2)]
        pT, pTb = sc.ps()
        pW = [sc.ps() for _ in range(2)]
        pA = [sc.ps() for _ in range(2)]
        v3 = lambda t: t[:].rearrange("p (h j) -> p h j", j=64)
        nj = 0
        for b in range(NBL):
            ZRd = Z.ZR[b]
            zrb = zb(f"ZR{b}")
            for tb in range(NTB):
                t0 = tb * 128
                kb.dma(Zc[:], ZRd[t0:t0 + 128, :], R=[zrb], W=[Zcb])
                if tb in (0, 2):
                    kb.op("pool", lambda g: g.memset(Zp[:], 0.0), W=[Zpb])
                    kb.dma(Zp[1:128, :], ZRd[t0:t0 + 127, :], R=[zrb], W=[Zpb])
                else:
                    kb.dma(Zp[:], ZRd[t0 - 1:t0 + 127, :], R=[zrb], W=[Zpb])
                if tb in (1, NTB - 1):
                    kb.op("pool", lambda g: g.memset(Zn[:], 0.0), W=[Znb])
                    kb.dma(Zn[0:127, :], ZRd[t0 + 1:t0 + 128, :], R=[zrb], W=[Znb])
                else:
                    kb.dma(Zn[:], ZRd[t0 + 1:t0 + 129, :], R=[zrb], W=[Znb])
                kb.tt("pool", Zp[:], Zp[:], Zc[:], ALU.subtract, R=[Zcb], W=[Zpb])
                kb.tt("pool", Zp[:], Zp[:], MU[0][0][:], ALU.mult, R=[MU[0][1]], W=[Zpb])
                kb.tt("dve", Zn[:], Zn[:], Zc[:], ALU.subtract, R=[Zcb], W=[Znb])
                kb.tt("dve", Zn[:], Zn[:], MU[1][0][:], ALU.mult, R=[MU[1][1]], W=[Znb])
                kb.tt("dve", Zc[:], Zc[:], Zp[:], ALU.add, R=[Zpb], W=[Zcb])
                kb.tt("dve", Zc[:], Zc[:], Zn[:], ALU.add, R=[Znb], W=[Zcb])
                for ci in range(7):
                    c0 = ci * 512
                    n = min(512, RIN - c0)
                    p, pb = pJ[nj % 2]
                    nj += 1
                    kb.mm([lambda e: e.matmul(p[:, :n], lhsT=C.J, rhs=Zc[:, c0:c0 + n], start=True, stop=True)], R=[Zcb, C.cstb], W=[pb])
                    if ci == 0:
                        kb.cp("act", Zr[:, c0:c0 + n], p[:, :n], R=[pb], W=[Zrb])
                    else:
                        kb.op("act" if ci % 2 == 0 else "dve",
                              (lambda g: g.copy(out=Zr[:, c0:c0 + n], in_=p[:, :n])) if ci % 2 == 0 else (lambda g: g.tensor_copy(out=Zr[:, c0:c0 + n], in_=p[:, :n])),
                              R=[pb], WA=[Zrb])
                kb.dma(Z.AUX[b][t0:t0 + 128, 1, :], Zc[:, 2048:3072], R=[Zcb], WA=[zb(f"AUX{b}")])
                import os
                DL = int(os.environ.get("DLEVEL", "9"))
                for d in range(2 if DL >= 2 else 0):
                    zs, zsb = (Zc, Zcb) if d == 0 else (Zr, Zrb)
                    s0 = t0 if d == 0 else rev_block(tb) * 128
                    row0 = sc_row(d, b)
                    ntr = 3 if d == 0 else 2
                    kb.mm([lambda e, i=i: e.transpose(pT[:, i * 128:(i + 1) * 128], zs[:, 3072 + i * 128:3200 + i * 128], C.ident) for i in range(ntr)],
                          R=[zsb, C.cstb], W=[pTb])
                    kb.act(wlT[:], pT[:, 0:128], AF.Tanh, R=[pTb], W=[wlTb])
                    kb.cp("act", alT[:], pT[:, 128:256], R=[pTb], W=[alTb])
                    if d == 0:
                        kb.act(glT[:], pT[:, 256:384], AF.Sigmoid, R=[pTb], W=[glTb])
                    for hf in range(2):
                        kb.mm([lambda e: e.matmul(pW[hf][0][:, :], lhsT=wlT[d * 64:(d + 1) * 64, :], rhs=W2[d * 64:(d + 1) * 64, hf * 512:(hf + 1) * 512], start=True, stop=True)],
                              R=[wlTb, W2b], W=[pW[hf][1]])
                        kb.mm([lambda e: e.matmul(pA[hf][0][:, :], lhsT=alT[d * 64:(d + 1) * 64, :], rhs=A2[d * 64:(d + 1) * 64, hf * 512:(hf + 1) * 512], start=True, stop=True)],
                              R=[alTb, A2b], W=[pA[hf][1]])
                    for hf in range(2):
                        sl = slice(hf * 512, (hf + 1) * 512)
                        if hf == 0:
                            kb.tt("dve", X1[:, sl], pW[hf][0][:, :], W0[d][0][:, sl], ALU.add, R=[pW[hf][1], W0[d][1]], W=[X1b])
                            kb.tt("dve", A_[:, sl], pA[hf][0][:, :], A0[d][0][:, sl], ALU.add, R=[pA[hf][1], A0[d][1]], W=[A_b])
                        else:
                            kb.op("dve", lambda g: g.tensor_tensor(out=X1[:, sl], in0=pW[hf][0][:, :], in1=W0[d][0][:, sl], op=ALU.add), R=[pW[hf][1], W0[d][1]], WA=[X1b])
                            kb.op("dve", lambda g: g.tensor_tensor(out=A_[:, sl], in0=pA[hf][0][:, :], in1=A0[d][0][:, sl], op=ALU.add), R=[pA[hf][1], A0[d][1]], WA=[A_b])
                    kb.act(X1[:], X1[:], AF.Sigmoid, R=[X1b], W=[X1b])
                    kb.act(A_[:], A_[:], AF.Sigmoid, R=[A_b], W=[A_b])
                    kb.act(DEC[:], X1[:], AF.Exp, R=[X1b], W=[DECb], scale=float(-np.exp(-0.5)))
                    k_ = zs[:, 1024:2048]
                    r_ = zs[:, 0:1024]
                    if DL < 3:
                        continue
                    kb.tt("pool", KKn[:], k_, KK[:], ALU.mult, R=[zsb, KKb], W=[KKnb])
                    kb.tt("pool", TM[:], KKn[:], KKn[:], ALU.mult, R=[KKnb], W=[TMb])
                    kb.red(ssq[:], v3(TM), ALU.add, R=[TMb], W=[ssqb])
                    kb.act(ssq[:], ssq[:], AF.Sqrt, R=[ssqb], W=[ssqb])
                    kb.ts("dve", ssq[:], ssq[:], 1e-12, ALU.max, R=[ssqb], W=[ssqb])
                    kb.op("dve", lambda g: g.reciprocal(out=ssq[:], in_=ssq[:]), R=[ssqb], W=[ssqb])
                    kb.tt("dve", v3(KKn), v3(KKn), ssq[:].unsqueeze(2).to_broadcast([128, 16, 64]), ALU.mult, R=[ssqb], W=[KKnb])
                    kb.tt("pool", Bt[:], KKn[:], A_[:], ALU.mult, R=[KKnb, A_b], W=[Btb])
                    kb.tt("pool", TM[:], A_[:], KA[:], ALU.mult, R=[A_b, KAb], W=[TMb])
                    kb.tt("pool", TM[:], TM[:], OM[:], ALU.add, R=[OMb], W=[TMb])
                    kb.tt("pool", KD[:], k_, TM[:], ALU.mult, R=[zsb, TMb], W=[KDb])
                    kb.tt("dve", TM[:], r_, RKt[:], ALU.mult, R=[zsb, RKtb, KDb], W=[TMb])
                    kb.tt("dve", TM[:], TM[:], KD[:], ALU.mult, R=[KDb], W=[TMb])
                    kb.red(rk[:], v3(TM), ALU.add, R=[TMb], W=[rkb])
                    if DL < 4:
                        continue
                    kb.dma(Z.RK[b][d, s0:s0 + 128, :], rk[:], R=[rkb], WA=[zb(f"RK{b}")])
                    fields = [(KKn[:], KKnb), (r_, zsb), (DEC[:], DECb), (Bt[:], Btb), (KD[:], KDb), (zs[:, 2048:3072], zsb)]
                    import os
                    for f, (ap, apb) in enumerate(fields):
                        if os.environ.get("SKIPSC"):
                            continue
                        kb.dma(Z.SC[row0:row0 + 16, s0:s0 + 128, f * 64:(f + 1) * 64].rearrange("h t j -> t h j"),
                               ap.rearrange("p (h j) -> p h j", j=64), R=[apb], WA=[zb("SC")])
                    if d == 0:
                        for hf in range(2):
                            kb.mm([lambda e: e.matmul(pW[hf][0][:, :], lhsT=glT[:, :], rhs=G2[:, hf * 512:(hf + 1) * 512], start=True, stop=True)],
                                  R=[glTb, G2b], W=[pW[hf][1]])
                        kb.cp("act", TG[:, 0:512], pW[0][0][:, :], R=[pW[0][1]], W=[TGb])
                        kb.op("act", lambda g: g.copy(out=TG[:, 512:1024], in_=pW[1][0][:, :]), R=[pW[1][1]], WA=[TGb])
                        kb.dma(Z.AUX[b][t0:t0 + 128, 0, :], TG[:], R=[TGb], WA=[zb(f"AUX{b}")])


def phaseE(kb, C, I, Z, zb, l, nsteps=T):
    CH = 32
    with kb.scope() as sc:
        St, Sb = sc.sb([128, 32, 64], F32, "S")
        ops = [sc.sb([128, CH, 320], F32, "ops") for _ in range(2)]
        vhs = [sc.sb([128, CH, 32], F32, "vh") for _ in range(2)]
        ybs = [sc.sb([128, CH, 32], F32, "yb") for _ in range(2)]
        T1, T1b = sc.sb([128, 32, 64], F32, "T1")
        T2, T2b = sc.sb([128, 32, 64], F32, "T2")
        P1s = [sc.sb([128, 32, 64], F32, "P1") for _ in range(2)]
        T4, T4b = sc.sb([128, 32, 64], F32, "T4")
        sa, sab = sc.sb([128, 32], F32, "sa")
        kb.op("dve", lambda g: g.memset(St[:], 0.0), W=[Sb])
        scb = zb("SC")
        step = 0
        for c in range(nsteps // CH):
            s0 = c * CH
            o, ob = ops[c % 2]
            vh, vhb = vhs[c % 2]
            yb, ybb = ybs[c % 2]
            for ih in range(2):
                if ih == 0:
                    kb.dma(o[0:64, :, :], Z.SC[:, s0:s0 + CH, 0:320], R=[scb], W=[ob])
                    kb.dma(vh[0:64, :, :], Z.SC[:, s0:s0 + CH, 320:352], R=[scb], W=[vhb])
                else:
                    kb.dma(o[64:128, :, :], Z.SC[:, s0:s0 + CH, 0:320], R=[scb], WA=[ob])
                    kb.dma(vh[64:128, :, :], Z.SC[:, s0:s0 + CH, 352:384], R=[scb], WA=[vhb])
            for s in range(CH):
                fb = lambda f: o[:, s, f * 64:(f + 1) * 64].unsqueeze(1).to_broadcast([128, 32, 64])
                P1, P1b = P1s[step % 2]
                step += 1
                kb.tt("pool", T4[:], vh[:, s, :].unsqueeze(2).to_broadcast([128, 32, 64]), fb(4), ALU.mult, R=[vhb, ob], W=[T4b])
                kb.tt("pool", P1[:], St[:], fb(2), ALU.mult, R=[Sb, ob], W=[P1b])
                kb.tt("pool", P1[:], P1[:], T4[:], ALU.add, R=[T4b], W=[P1b])
                kb.tt("dve", T1[:], St[:], fb(0), ALU.mult, R=[Sb, ob], W=[T1b])
                kb.red(sa[:], T1[:], ALU.add, R=[T1b], W=[sab])
                kb.tt("dve", T2[:], sa[:].unsqueeze(2).to_broadcast([128, 32, 64]), fb(3), ALU.mult, R=[sab, ob], W=[T2b])
                kb.tt("dve", St[:], P1[:], T2[:], ALU.subtract, R=[P1b, T2b], W=[Sb])
                kb.tt("dve", T1[:], St[:], fb(1), ALU.mult, R=[Sb, ob], W=[T1b])
                if s == 0:
                    kb.red(yb[:, s, :], T1[:], ALU.add, R=[T1b], W=[ybb])
                else:
                    kb.op("dve", lambda g: g.tensor_reduce(out=yb[:, s, :], in_=T1[:], axis=AX.X, op=ALU.add), R=[T1b], WA=[ybb])
            for ih in range(2):
                kb.dma(Z.YS[:, s0:s0 + CH, ih * 32:(ih + 1) * 32], yb[ih * 64:(ih + 1) * 64, :, :], R=[ybb], WA=[zb("YS")])


def phaseF(kb, C, I, Z, zb, l, b, last):
    with kb.scope() as sc:
        bc = lambda ap: ap.partition_broadcast(128)
        LNG, LNGb = sc.sb([128, 1024], F32, "lng")
        LNB, LNBb = sc.sb([128, 1024], F32, "lnb")
        kb.dma(LNG[:], bc(I.rwkv_vecs[l, 3, :]), W=[LNGb])
        kb.dma(LNB[:], bc(I.rwkv_vecs[l, 4, :]), W=[LNBb])
        idb, idbb = sc.sb([128, 128], BF16, "idb")
        kb.cp("dve", idb[:], C.ident, R=[C.cstb], W=[idbb])
        YF, YFb = sc.sb([128, 1024], F32, "yf")
        YR, YRb = sc.sb([128, 1040], F32, "yr")
        RK0, RK0b = sc.sb([128, 16], F32, "rk0")
        AX_, AXb = sc.sb([128, 2, 1024], F32, "aux")
        Y, Yb = sc.sb([128, 1024], F32, "y")
        SQ, SQb = sc.sb([128, 1024], F32, "sq")
        m16, m16b = sc.sb([128, 16], F32, "m16")
        v16, v16b = sc.sb([128, 16], F32, "v16")
        rks, rksb = sc.sb([128, 16], F32, "rks")
        YO, YOb = sc.sb([128, 1024], BF16, "yo")
        stg, stgb = sc.sb([128, 8, 128], BF16, "stg")
        pFa, pFab = sc.ps()
        pFb, pFbb = sc.ps()
        pFc, pFcb = sc.ps()
        pTr, pTrb = sc.ps([128, 1024], BF16)
        v3 = lambda ap: ap.rearrange("p (h j) -> p h j", j=64)
        b16 = lambda t: t[:].unsqueeze(2).to_broadcast([128, 16, 64])
        for tb in range(2 if last else 0, NTB):
            t0 = tb * 128
            s0r = rev_block(tb) * 128
            kb.dma(v3(YF[:]), Z.YS[sc_row(0, b):sc_row(0, b) + 16, t0:t0 + 128, :].rearrange("h t i -> t h i"), R=[zb("YS")], W=[YFb])
            kb.dma(v3(YR[:, 0:1024]), Z.YS[sc_row(1, b):sc_row(1, b) + 16, s0r:s0r + 128, :].rearrange("h t i -> t h i"), R=[zb("YS")], W=[YRb])
            kb.dma(YR[:, 1024:1040], Z.RK[b][1, s0r:s0r + 128, :], R=[zb(f"RK{b}")], WA=[YRb])
            kb.dma(RK0[:], Z.RK[b][0, t0:t0 + 128, :], R=[zb(f"RK{b}")], W=[RK0b])
            kb.dma(AX_[:], Z.AUX[b][t0:t0 + 128, :, :], R=[zb(f"AUX{b}")], W=[AXb])
            kb.mm([lambda e: e.matmul(pFa[:, :], lhsT=C.J, rhs=YR[:, 0:512], start=True, stop=True)], R=[YRb, C.cstb], W=[pFab])
            kb.mm([lambda e: e.matmul(pFb[:, :], lhsT=C.J, rhs=YR[:, 512:1024], start=True, stop=True)], R=[YRb, C.cstb], W=[pFbb])
            kb.mm([lambda e: e.matmul(pFc[:, 0:16], lhsT=C.J, rhs=YR[:, 1024:1040], start=True, stop=True)], R=[YRb, C.cstb], W=[pFcb])
            kb.tt("dve", Y[:, 0:512], YF[:, 0:512], pFa[:, :], ALU.add, R=[YFb, pFab], W=[Yb])
            kb.op("dve", lambda g: g.tensor_tensor(out=Y[:, 512:1024], in0=YF[:, 512:1024], in1=pFb[:, :], op=ALU.add), R=[YFb, pFbb], WA=[Yb])
            kb.tt("dve", rks[:], RK0[:], pFc[:, 0:16], ALU.add, R=[RK0b, pFcb], W=[rksb])
            kb.red(m16[:], v3(Y[:]), ALU.add, R=[Yb], W=[m16b])
            kb.ts("dve", m16[:], m16[:], 1.0 / 64, ALU.mult, R=[m16b], W=[m16b])
            kb.tt("dve", v3(Y[:]), v3(Y[:]), b16(m16), ALU.subtract, R=[m16b], W=[Yb])
            kb.tt("pool", SQ[:], Y[:], Y[:], ALU.mult, R=[Yb], W=[SQb])
            kb.red(v16[:], v3(SQ[:]), ALU.add, R=[SQb], W=[v16b])
            kb.act(v16[:], v16[:], AF.Sqrt, R=[v16b, C.cstb], W=[v16b], bias=C.cst[:, 513:514], scale=1.0 / 64)
            kb.op("dve", lambda g: g.reciprocal(out=v16[:], in_=v16[:]), R=[v16b], W=[v16b])
            kb.tt("dve", v3(Y[:]), v3(Y[:]), b16(v16), ALU.mult, R=[v16b], W=[Yb])
            kb.tt("pool", Y[:], Y[:], LNG[:], ALU.mult, R=[LNGb], W=[Yb])
            kb.tt("pool", Y[:], Y[:], LNB[:], ALU.add, R=[LNBb], W=[Yb])
            kb.tt("dve", v3(SQ[:]), v3(AX_[:, 1, :]), b16(rks), ALU.mult, R=[AXb, rksb], W=[SQb])
            kb.tt("dve", Y[:], Y[:], SQ[:], ALU.add, R=[SQb], W=[Yb])
            kb.tt("dve", YO[:], Y[:], AX_[:, 0, :], ALU.mult, R=[Yb, AXb], W=[YOb])
            kb.mm([lambda e, j=j: e.transpose(pTr[:, j * 128:(j + 1) * 128], YO[:, j * 128:(j + 1) * 128], idb[:]) for j in range(8)],
                  R=[YOb, idbb], W=[pTrb])
            kb.cp("act", stg[:], pTr[:, :].rearrange("p (c t) -> p c t", t=128), R=[pTrb], W=[stgb])
            kb.dma(pm(Z.RWO[b])[:, :, t0:t0 + 128], stg[:], R=[stgb], WA=[zb(f"RWO{b}")])

def phaseG(kb, C, I, Z, zb, l, b, last):
    with kb.scope() as sc:
        CV, CVbs = sc.sb([128, 8, T], F32, "cv", nb=8)
        UH = [sc.sb([128, S + 30], F32, "uh") for _ in range(2)]
        cw, cwb = sc.sb([128, 8, 31], F32, "cw")
        cvec, cvecb = sc.sb([128, 3, 8], F32, "cvec")
        kb.dma(cw[:], I.conv_w[l], W=[cwb])
        kb.dma(cvec[:], I.conv_vecs[l], W=[cvecb])
        segs = [(CL, S)] if last else [(0, CL), (CL, S)]
        n = 0
        for cc in range(8):
            e = "dve"
            for (t0, W_) in segs:
                uh, uhb = UH[n % 2]
                n += 1
                kb.op(e, lambda g: g.memset(uh[:, 0:15], 0.0), W=[uhb])
                kb.op(e, lambda g: g.memset(uh[:, 15 + W_:30 + W_], 0.0), WA=[uhb])
                kb.dma(uh[:, 15:15 + W_], Z.CU[b][cc * 128:(cc + 1) * 128, t0:t0 + W_], R=[zb(f"CU{b}")], W=[uhb])
                dst = CV[:, cc, t0:t0 + W_]
                kb.ts(e, dst, uh[:, 0:W_], cw[:, cc, 0:1], ALU.mult, cvec[:, 0, cc:cc + 1], ALU.add, R=[uhb, cwb, cvecb], W=[CVbs[cc]])
                for k in range(1, 31):
                    kb.stt(e, dst, uh[:, k:k + W_], cw[:, cc, k:k + 1], dst, ALU.mult, ALU.add, R=[uhb, cwb], W=[CVbs[cc]])
        sq, sqbs = sc.sb([128, 8, 256], F32, "sq", nb=8)
        ps1, ps1b = sc.ps()
        ps2, ps2b = sc.ps()
        mean, meanb = sc.sb([128, 256], F32, "mean")
        rstd, rstdb = sc.sb([128, 256], F32, "rstd")
        nmr, nmrb = sc.sb([128, 256], F32, "nmr")
        stg = [sc.sb([128, 8, 256], BF16, "stg", nb=8) for _ in range(2)]
        for ti in range(1 if last else 0, T // 256):
            t0 = ti * 256
            x = CV[:, :, t0:t0 + 256]
            ln_stats(kb, C, x, CVbs, 8, 256, sq, sqbs, ps1, ps1b, ps2, ps2b, mean, meanb, rstd, rstdb, nmr, nmrb, 1024, 0)
            st, stbs = stg[ti % 2]
            for kc in range(8):
                e = "dve" if kc % 2 == 0 else "pool"
                t = x[:, kc, :]
                kb.tt(e, t, t, rstd[:, :], ALU.mult, R=[rstdb], W=[CVbs[kc]])
                kb.tt(e, t, t, nmr[:, :], ALU.add, R=[nmrb], W=[CVbs[kc]])
                kb.act(st[:, kc, :], t, AF.Silu, R=[CVbs[kc], cvecb], W=[stbs[kc]], scale=cvec[:, 1, kc:kc + 1], bias=cvec[:, 2, kc:kc + 1])
            kb.dma(pm(Z.CVO[b])[:, :, t0:t0 + 256], st[:], R=stbs, WA=[zb(f"CVO{b}")])


def phaseH(kb, C, I, Z, zb, l, b, last):
    tiles = TILES_LAT if last else TILES
    with kb.scope() as sc:
        A3 = [sc.sb([128, 8, T], BF16, "a3") for _ in range(3)]
        srcs = [(Z.ATT[b], f"ATT{b}"), (Z.RWO[b], f"RWO{b}"), (Z.CVO[b], f"CVO{b}")]
        c0 = CL if last else 0
        for br in range(3):
            kb.dma(A3[br][0][:, :, c0:T], pm(srcs[br][0])[:, :, c0:T], R=[zb(srcs[br][1])], W=[A3[br][1]])
        Wd = [I.w_att_o, I.w_rwkv_o, I.w_conv_o]
        wts = [[sc.sb([128, 8, 512], BF16, "wbr") for _ in range(3)] for _ in range(2)]
        gts = [sc.sb([128, 3, 512], BF16, "gt") for _ in range(2)]
        tas = [sc.sb([128, 512], F32, "ta") for _ in range(2)]
        tbs = [sc.sb([128, 512], F32, "tb") for _ in range(2)]
        tcs = [sc.sb([128, 512], F32, "tc") for _ in range(2)]
        mts = [sc.sb([128, 512], BF16, "mt") for _ in range(2)]
        pss = [[sc.ps() for _ in range(3)] for _ in range(2)]
        Gv = Z.G[b].rearrange("(br m p) t -> p br m t", br=3, p=128)
        n = 0
        for og in range(4):
            ws = wts[og % 2]
            for br in range(3):
                kb.dma(ws[br][0][:], pm(Wd[br][l, :, og * 512:(og + 1) * 512]), W=[ws[br][1]], q="pool")
            for j in range(4):
                mc = og * 4 + j
                for (t0, W_) in tiles:
                    i2 = n % 2
                    n += 1
                    gt, gtb = gts[i2]
                    kb.dma(gt[:, :, :W_], Gv[:, :, mc, t0:t0 + W_], R=[zb(f"G{b}")], W=[gtb])
                    P = pss[i2]
                    for br in range(3):
                        kb.mm([lambda e, kc=kc, br=br: e.matmul(P[br][0][:, :W_], lhsT=ws[br][0][:, kc, j * 128:(j + 1) * 128], rhs=A3[br][0][:, kc, t0:t0 + W_],
                                                               start=(kc == 0), stop=(kc == 7)) for kc in range(8)], R=[ws[br][1], A3[br][1]], W=[P[br][1]])
                    ta, tab = tas[i2]
                    tb_, tbb = tbs[i2]
                    tc, tcb = tcs[i2]
                    mt, mtb = mts[i2]
                    kb.tt("dve", ta[:, :W_], P[0][0][:, :W_], gt[:, 0, :W_], ALU.mult, R=[P[0][1], gtb], W=[tab])
                    kb.tt("dve", tb_[:, :W_], P[1][0][:, :W_], gt[:, 1, :W_], ALU.mult, R=[P[1][1], gtb], W=[tbb])
                    kb.tt("dve", tc[:, :W_], P[2][0][:, :W_], gt[:, 2, :W_], ALU.mult, R=[P[2][1], gtb], W=[tcb])
                    kb.tt("pool", ta[:, :W_], ta[:, :W_], tb_[:, :W_], ALU.add, R=[tbb], W=[tab])
                    kb.tt("pool", mt[:, :W_], ta[:, :W_], tc[:, :W_], ALU.add, R=[tab, tcb], W=[mtb])
                    kb.dma(Z.MT[b][mc * 128:(mc + 1) * 128, t0:t0 + W_], mt[:, :W_], R=[mtb], WA=[zb(f"MT{b}")])


def phaseI(kb, C, I, Z, zb, l, b, last):
    tiles = TILES_LAT if last else TILES
    with kb.scope() as sc:
        MTr, MTrb = sc.sb([128, KC, T], BF16, "mtr")
        c0 = CL if last else 0
        kb.dma(MTr[:, :, c0:T], pm(Z.MT[b])[:, :, c0:T], R=[zb(f"MT{b}")], W=[MTrb])
        wts = [sc.sb([128, KC, 512], BF16, "wout") for _ in range(2)]
        xts = [sc.sb([128, 512], F32, "xt") for _ in range(3)]
        yas = [sc.sb([128, 512], F32, "ya") for _ in range(3)]
        pss = [sc.ps() for _ in range(4)]
        n = 0
        for og in range(4):
            w, wb = wts[og % 2]
            kb.dma(w[:], pm(I.w_out[l, :, og * 512:(og + 1) * 512]), W=[wb], q="pool")
            for j in range(4):
                mc = og * 4 + j
                for (t0, W_) in tiles:
                    r = 2 if t0 < CL else b
                    p, pb = pss[n % 4]
                    xt, xtb = xts[n % 3]
                    ya, yab = yas[n % 3]
                    n += 1
                    kb.dma(xt[:, :W_], Z.XT[b][mc * 128:(mc + 1) * 128, t0:t0 + W_], R=[zb(f"XT{b}")], W=[xtb])
                    kb.mm([lambda e, kc=kc: e.matmul(p[:, :W_], lhsT=w[:, kc, j * 128:(j + 1) * 128], rhs=MTr[:, kc, t0:t0 + W_],
                                                     start=(kc == 0), stop=(kc == KC - 1)) for kc in range(KC)], R=[wb, MTrb], W=[pb])
                    kb.act(ya[:, :W_], p[:, :W_], AF.Copy, R=[pb, C.MODb], W=[yab], scale=C.MOD[:, l, 32 + mc, r:r + 1])
                    kb.stt("dve", ya[:, :W_], xt[:, :W_], DN_ALPHA, ya[:, :W_], ALU.mult, ALU.add, R=[xtb], W=[yab])
                    kb.dma(Z.RT[b][mc * 128:(mc + 1) * 128, t0:t0 + W_], ya[:, :W_], R=[yab], WA=[zb(f"RT{b}")])


def phaseJ(kb, C, I, Z, zb, l, b, last):
    with kb.scope() as sc:
        xs = [sc.sb([128, KC, 256], F32, "xs", nb=KC) for _ in range(2)]
        x1s = [sc.sb([128, KC, 256], F32, "x1", nb=KC) for _ in range(2)]
        hf, hfbs = sc.sb([128, KC, 256], F32, "hf", nb=KC)
        hb, hbbs = sc.sb([128, KC, 256], BF16, "hb", nb=KC)
        sq, sqbs = sc.sb([128, KC, 256], F32, "sq", nb=KC)
        ps1, ps1b = sc.ps()
        ps2, ps2b = sc.ps()
        psl, pslb = sc.ps()
        pst, pstb = sc.ps()
        mean, meanb = sc.sb([128, 256], F32, "mean")
        rstd, rstdb = sc.sb([128, 256], F32, "rstd")
        nmr, nmrb = sc.sb([128, 256], F32, "nmr")
        lnv, lnvb = sc.sb([128, 4, KC], F32, "lnv")
        kb.dma(lnv[:], I.ln_vecs[l], W=[lnvb])
        wgr, wgrb = sc.sb([128, KC, 36], F32, "wgr")
        kb.dma(wgr[:], pm(I.w_gr[l]), W=[wgrb])
        bgr, bgrb = sc.sb([128, 36], F32, "bgr")
        kb.dma(bgr[:], I.b_gr[l].partition_broadcast(128), W=[bgrb])
        lg, lgb = sc.sb([128, 36], F32, "lg")
        sm = {nm: sc.sb([128, 8], F32, nm) for nm in ("ig", "pe", "oh1", "p2", "oh2", "wt8", "goh", "gex")}
        s1 = {nm: sc.sb([128, 1], F32, nm) for nm in ("gmax", "ngmax", "gsum", "emax", "nemax", "m1", "m2", "den")}
        wts_, wtsb = sc.sb([128, 32], F32, "wts")
        wtT, wtTb = sc.sb([32, 128], F32, "wtT")
        xtv = pm(Z.XT[b])
        rtv = pm(Z.RT[b])
        h2v = pm(Z.H2[b])
        for ti in range(1 if last else 0, T // 256):
            t0 = ti * 256
            r = 2 if t0 < CL else b
            x, xbs = xs[ti % 2]
            x1, x1bs = x1s[ti % 2]
            kb.dma(x[:], rtv[:, :, t0:t0 + 256], R=[zb(f"RT{b}")], W=xbs)
            ln_stats(kb, C, x, xbs, KC, 256, sq, sqbs, ps1, ps1b, ps2, ps2b, mean, meanb, rstd, rstdb, nmr, nmrb, D, 0)
            ln_apply(kb, x, xbs, KC, 256, rstd, rstdb, nmr, nmrb, lambda kc: x1[:, kc, :], x1bs,
                     lambda kc: lnv[:, 0, kc:kc + 1], lambda kc: lnv[:, 1, kc:kc + 1], extraR=[lnvb])
            kb.dma(xtv[:, :, t0:t0 + 256], x1[:], R=x1bs, WA=[zb(f"XT{b}")])
            ln_stats(kb, C, x1, x1bs, KC, 256, sq, sqbs, ps1, ps1b, ps2, ps2b, mean, meanb, rstd, rstdb, nmr, nmrb, D, 0)
            ln_apply(kb, x1, x1bs, KC, 256, rstd, rstdb, nmr, nmrb, lambda kc: hf[:, kc, :], hfbs,
                     lambda kc: C.MOD[:, l, 64 + kc, r:r + 1], lambda kc: C.MOD[:, l, 48 + kc, r:r + 1], extraR=[C.MODb])
            for kc in range(KC):
                kb.cp("act" if kc % 2 == 0 else "pool", hb[:, kc, :], hf[:, kc, :], R=[hfbs[kc]], W=[hbbs[kc]])
            kb.dma(h2v[:, :, t0:t0 + 256], hb[:], R=hbbs, WA=[zb(f"H2{b}")])
            for sub in range(2):
                kb.mm([lambda e, kc=kc: e.matmul(psl[:, 0:36], lhsT=hf[:, kc, sub * 128:(sub + 1) * 128], rhs=wgr[:, kc, :], start=(kc == 0), stop=(kc == KC - 1))
                       for kc in range(KC)], R=list(hfbs) + [wgrb], W=[pslb])
                kb.tt("dve", lg[:], psl[:, 0:36], bgr[:], ALU.add, R=[pslb, bgrb], W=[lgb])
                T_ = lambda nm: sm[nm][0]
                B_ = lambda nm: sm[nm][1]
                t1 = lambda nm: s1[nm][0]
                b1 = lambda nm: s1[nm][1]
                gl = lg[:, 0:4]
                el = lg[:, 4:36].rearrange("p (g e) -> p g e", e=8)
                kb.op("dve", lambda g: g.tensor_reduce(out=t1("gmax")[:], in_=gl, axis=AX.X, op=ALU.max), R=[lgb], W=[b1("gmax")])
                kb.ts("dve", T_("goh")[:, 0:4], gl, t1("gmax")[:, 0:1], ALU.is_equal, R=[lgb, b1("gmax")], W=[B_("goh")])
                kb.ts("dve", t1("ngmax")[:], t1("gmax")[:], -1.0, ALU.mult, R=[b1("gmax")], W=[b1("ngmax")])
                kb.act(T_("gex")[:, 0:4], gl, AF.Exp, R=[lgb, b1("ngmax")], W=[B_("gex")], bias=t1("ngmax")[:, 0:1], scale=1.0)
                kb.red(t1("gsum")[:], T_("gex")[:, 0:4], ALU.add, R=[B_("gex")], W=[b1("gsum")])
                kb.ts("dve", T_("ig")[:], el[:, 0, :], T_("goh")[:, 0:1], ALU.mult, R=[lgb, B_("goh")], W=[B_("ig")])
                for g_ in range(1, 4):
                    kb.stt("dve", T_("ig")[:], el[:, g_, :], T_("goh")[:, g_:g_ + 1], T_("ig")[:], ALU.mult, ALU.add, R=[lgb, B_("goh")], W=[B_("ig")])
                kb.op("dve", lambda g: g.tensor_reduce(out=t1("emax")[:], in_=T_("ig")[:], axis=AX.X, op=ALU.max), R=[B_("ig")], W=[b1("emax")])
                kb.ts("dve", T_("oh1")[:], T_("ig")[:], t1("emax")[:, 0:1], ALU.is_equal, R=[B_("ig"), b1("emax")], W=[B_("oh1")])
                kb.ts("dve", t1("nemax")[:], t1("emax")[:], -1.0, ALU.mult, R=[b1("emax")], W=[b1("nemax")])
                kb.act(T_("pe")[:], T_("ig")[:], AF.Exp, R=[B_("ig"), b1("nemax")], W=[B_("pe")], bias=t1("nemax")[:, 0:1], scale=1.0)
                kb.op("dve", lambda g: g.tensor_reduce(out=t1("m1")[:], in_=T_("pe")[:], axis=AX.X, op=ALU.max), R=[B_("pe")], W=[b1("m1")])
                kb.stt("dve", T_("p2")[:], T_("oh1")[:], -4.0, T_("pe")[:], ALU.mult, ALU.add, R=[B_("oh1"), B_("pe")], W=[B_("p2")])
                kb.op("dve", lambda g: g.tensor_reduce(out=t1("m2")[:], in_=T_("p2")[:], axis=AX.X, op=ALU.max), R=[B_("p2")], W=[b1("m2")])
                kb.ts("dve", T_("oh2")[:], T_("p2")[:], t1("m2")[:, 0:1], ALU.is_equal, R=[B_("p2"), b1("m2")], W=[B_("oh2")])
                kb.tt("dve", T_("oh1")[:], T_("oh1")[:], T_("oh2")[:], ALU.add, R=[B_("oh2")], W=[B_("oh1")])
                kb.tt("dve", T_("wt8")[:], T_("oh1")[:], T_("pe")[:], ALU.mult, R=[B_("oh1"), B_("pe")], W=[B_("wt8")])
                kb.tt("dve", t1("den")[:], t1("m1")[:], t1("m2")[:], ALU.add, R=[b1("m1"), b1("m2")], W=[b1("den")])
                kb.tt("dve", t1("den")[:], t1("den")[:], t1("gsum")[:], ALU.mult, R=[b1("gsum")], W=[b1("den")])
                kb.op("dve", lambda g: g.reciprocal(out=t1("den")[:], in_=t1("den")[:]), R=[b1("den")], W=[b1("den")])
                kb.ts("dve", T_("wt8")[:], T_("wt8")[:], t1("den")[:, 0:1], ALU.mult, R=[b1("den")], W=[B_("wt8")])
                for g_ in range(4):
                    kb.op("dve", lambda g: g.tensor_scalar(out=wts_[:, g_ * 8:(g_ + 1) * 8], in0=T_("wt8")[:], scalar1=T_("goh")[:, g_:g_ + 1], scalar2=None, op0=ALU.mult),
                          R=[B_("wt8"), B_("goh")], W=[wtsb] if g_ == 0 else [], WA=[] if g_ == 0 else [wtsb])
                kb.mm([lambda e: e.transpose(pst[0:32, 0:128], wts_[:, :], C.ident)], R=[wtsb, C.cstb], W=[pstb])
                kb.cp("act", wtT[:], pst[0:32, 0:128], R=[pstb], W=[wtTb])
                col = b * T + t0 + sub * 128
                kb.dma(Z.WTT[:, col:col + 128], wtT[:], R=[wtTb], WA=[zb("WTT")])


def phaseK(kb, C, I, Z, zb, l, last):
    if last:
        passes = [(b, t0, [(0, 512), (512, 512)]) for b in range(NBL) for t0 in (CL, CL + 1024)]
        PW = 1024
    else:
        passes = [(b, t0, [(0, 384), (384, 384), (768, 384)]) for b in range(NBL) for t0 in (0, 1152)]
        PW = 1152
    for (b, t0, subs) in passes:
        r_of = lambda tt: 2 if tt < CL else b
        with kb.scope() as sc:
            Y, Ybs = sc.sb([128, KC, PW], F32, "yacc", nb=KC)
            with kb.scope() as s1:
                H2r, H2rb = s1.sb([128, KC, PW], BF16, "h2r")
                kb.dma(H2r[:], pm(Z.H2[b])[:, :, t0:t0 + PW], R=[zb(f"H2{b}")], W=[H2rb])
                HD, HDbs = s1.sb([128, 8, PW], BF16, "hdn", nb=8)
                wtb, wtbb = s1.sb([128, PW], F32, "wtb")
                wg = [s1.sb([128, KC, 256], BF16, "wg") for _ in range(2)]
                wu = [s1.sb([128, KC, 256], BF16, "wu") for _ in range(2)]
                wd = [s1.sb([128, 8, 512], BF16, "wd") for _ in range(2)]
                sgs = [s1.sb([128, 384 if not last else 512], F32, "sg") for _ in range(2)]
                psg = [s1.ps() for _ in range(2)]
                psu = [s1.ps() for _ in range(2)]
                psd = [s1.ps() for _ in range(2)]
                ng = 0
                nd = 0
                nw = 0
                nwd = 0
                for e_ in range(NE):
                    col = b * T + t0
                    kb.dma(wtb[:], Z.WTT[e_, col:col + PW].partition_broadcast(128), R=[zb("WTT")], W=[wtbb])
                    for fg in range(4):
                        g_, gb_ = wg[nw % 2]
                        u_, ub_ = wu[nw % 2]
                        nw += 1
                        kb.dma(g_[:], pm(I.w_e_gate[l, e_, :, fg * 256:(fg + 1) * 256]), W=[gb_], q="pool")
                        kb.dma(u_[:], pm(I.w_e_up[l, e_, :, fg * 256:(fg + 1) * 256]), W=[ub_], q="pool")
                        for fj in range(2):
                            fc = fg * 2 + fj
                            for si, (o_, W_) in enumerate(subs):
                                pg, pgb = psg[ng % 2]
                                pu, pub = psu[ng % 2]
                                sg, sgb = sgs[ng % 2]
                                ng += 1
                                kb.mm([lambda e, kc=kc: e.matmul(pg[:, :W_], lhsT=g_[:, kc, fj * 128:(fj + 1) * 128], rhs=H2r[:, kc, o_:o_ + W_],
                                                                 start=(kc == 0), stop=(kc == KC - 1)) for kc in range(KC)], R=[gb_, H2rb], W=[pgb])
                                kb.mm([lambda e, kc=kc: e.matmul(pu[:, :W_], lhsT=u_[:, kc, fj * 128:(fj + 1) * 128], rhs=H2r[:, kc, o_:o_ + W_],
                                                                 start=(kc == 0), stop=(kc == KC - 1)) for kc in range(KC)], R=[ub_, H2rb], W=[pub])
                                kb.act(sg[:, :W_], pg[:, :W_], AF.Silu, R=[pgb], W=[sgb])
                                kb.tt("dve", sg[:, :W_], pu[:, :W_], sg[:, :W_], ALU.mult, R=[pub], W=[sgb])
                                if si == 0:
                                    kb.tt("pool", HD[:, fc, o_:o_ + W_], sg[:, :W_], wtb[:, o_:o_ + W_], ALU.mult, R=[sgb, wtbb], W=[HDbs[fc]])
                                else:
                                    kb.op("pool", lambda g: g.tensor_tensor(out=HD[:, fc, o_:o_ + W_], in0=sg[:, :W_], in1=wtb[:, o_:o_ + W_], op=ALU.mult),
                                          R=[sgb, wtbb], WA=[HDbs[fc]])
                    for og in range(4):
                        d_, db_ = wd[nwd % 2]
                        nwd += 1
                        kb.dma(d_[:], pm(I.w_e_down[l, e_, :, og * 512:(og + 1) * 512]), W=[db_], q="pool")
                        for j in range(4):
                            mc = og * 4 + j
                            for si, (o_, W_) in enumerate(subs):
                                pd, pdb = psd[nd % 2]
                                nd += 1
                                kb.mm([lambda e, fc=fc: e.matmul(pd[:, :W_], lhsT=d_[:, fc, j * 128:(j + 1) * 128], rhs=HD[:, fc, o_:o_ + W_],
                                                                 start=(fc == 0), stop=(fc == 7)) for fc in range(8)], R=[db_] + list(HDbs), W=[pdb])
                                if e_ == 0:
                                    if si == 0:
                                        kb.cp("dve", Y[:, mc, o_:o_ + W_], pd[:, :W_], R=[pdb], W=[Ybs[mc]])
                                    else:
                                        kb.op("dve", lambda g: g.tensor_copy(out=Y[:, mc, o_:o_ + W_], in_=pd[:, :W_]), R=[pdb], WA=[Ybs[mc]])
                                else:
                                    kb.tt("dve", Y[:, mc, o_:o_ + W_], Y[:, mc, o_:o_ + W_], pd[:, :W_], ALU.add, R=[pdb], W=[Ybs[mc]])
            with kb.scope() as s2:
                xt, xtbs = s2.sb([128, KC, 256], F32, "x2", nb=KC)
                sq, sqbs = s2.sb([128, KC, 256], F32, "sq", nb=KC)
                ps1, ps1b = s2.ps()
                ps2, ps2b = s2.ps()
                mean, meanb = s2.sb([128, 256], F32, "mean")
                rstd, rstdb = s2.sb([128, 256], F32, "rstd")
                nmr, nmrb = s2.sb([128, 256], F32, "nmr")
                lnv, lnvb = s2.sb([128, 4, KC], F32, "lnv")
                kb.dma(lnv[:], I.ln_vecs[l], W=[lnvb])
                xtv = pm(Z.XT[b])
                o_ = 0
                while o_ < PW:
                    W_ = min(256, PW - o_)
                    if t0 + o_ < CL < t0 + o_ + W_:
                        W_ = CL - (t0 + o_)
                    r = r_of(t0 + o_)
                    kb.dma(xt[:, :, :W_], xtv[:, :, t0 + o_:t0 + o_ + W_], R=[zb(f"XT{b}")], W=xtbs)
                    for kc in range(KC):
                        e = "dve" if kc % 2 == 0 else "pool"
                        yv = Y[:, kc, o_:o_ + W_]
                        kb.act(yv, yv, AF.Copy, R=[C.MODb], W=[Ybs[kc]], scale=C.MOD[:, l, 80 + kc, r:r + 1])
                        kb.stt("dve", xt[:, kc, :W_], xt[:, kc, :W_], DN_ALPHA, yv, ALU.mult, ALU.add, R=[Ybs[kc]], W=[xtbs[kc]])
                    ln_stats(kb, C, xt, xtbs, KC, W_, sq, sqbs, ps1, ps1b, ps2, ps2b, mean, meanb, rstd, rstdb, nmr, nmrb, D, 0)
                    ln_apply(kb, xt, xtbs, KC, W_, rstd, rstdb, nmr, nmrb, lambda kc: xt[:, kc, :W_], xtbs,
                             lambda kc: lnv[:, 2, kc:kc + 1], lambda kc: lnv[:, 3, kc:kc + 1], extraR=[lnvb])
                    kb.dma(xtv[:, :, t0 + o_:t0 + o_ + W_], xt[:, :, :W_], R=xtbs, WA=[zb(f"XT{b}")])
                    o_ += W_

def lay_pf(v, n):
    sh = v.shape[:-1]
    return np.ascontiguousarray(np.swapaxes(v.reshape(*sh, n, 128), -1, -2))


def host_consts():
    c = np.zeros((128, 5 * 128), np.float32)
    c[:, 0:128] = np.eye(128)
    c[:, 128:256] = np.eye(128)[::-1]
    c[:, 256:384] = 1.0
    P = np.zeros((128, 128), np.float32)
    for blk in range(2):
        for i in range(32):
            P[blk * 64 + i, blk * 64 + i + 32] = -1.0
            P[blk * 64 + i + 32, blk * 64 + i] = 1.0
    c[:, 384:512] = P.T
    c[:, 512] = LN_EPS
    c[:, 513] = GN_EPS
    c[:, 514] = 128 * RMS_EPS
    inv = (10000.0 ** (-np.arange(32, dtype=np.float32) / 32)).astype(np.float32)
    t = np.arange(S)
    row = (t // 64).astype(np.float32)
    col = (t % 64).astype(np.float32)
    ang = np.zeros((128, S), np.float32)
    for dd in range(128):
        blk = dd // 64
        f = dd % 32
        ang[dd] = (row if blk == 0 else col) * inv[f]
    cos = np.ones((128, T), np.float32)
    sin = np.zeros((128, T), np.float32)
    cos[:, CL:] = np.cos(ang)
    sin[:, CL:] = np.sin(ang)
    return c, cos, sin


def make_in_maps(inp, ncores=8, small=False):
    c, cos, sin = host_consts()
    f = lambda a: np.ascontiguousarray(np.asarray(a, dtype=np.float32))
    shared = {
        "w_mod": f(inp["w_mod"]), "b_mod": lay_pf(f(inp["b_mod"]), 96), "w_in": f(inp["w_in"]),
        "b_gate": lay_pf(f(inp["b_gate"]), 48),
        "qk_norm": np.ascontiguousarray(np.stack([f(inp["q_norm"]), f(inp["k_norm"])], -1)),
        "w_att_o": f(inp["w_att_o"]), "rwkv_mu": f(inp["rwkv_mu"]), "rwkv_w0": f(inp["rwkv_w0"]),
        "rwkv_w2": f(inp["rwkv_w2"]).reshape(DEPTH, 128, 1024), "rwkv_a0": f(inp["rwkv_a0"]),
        "rwkv_a2": f(inp["rwkv_a2"]).reshape(DEPTH, 128, 1024), "rwkv_g2": f(inp["rwkv_g2"]),
        "rwkv_vecs": np.ascontiguousarray(np.stack([f(inp["rwkv_k_k"]), f(inp["rwkv_k_a"]), f(inp["rwkv_r_k"]).reshape(DEPTH, 1024),
                                                    f(inp["rwkv_ln_g"]), f(inp["rwkv_ln_b"])], 1)),
        "w_rwkv_o": f(inp["w_rwkv_o"]),
        "conv_w": np.ascontiguousarray(np.transpose(f(inp["conv_w"]).reshape(DEPTH, 31, 8, 128), (0, 3, 2, 1))),
        "conv_vecs": np.ascontiguousarray(np.stack([lay_pf(f(inp["conv_b"]), 8), lay_pf(f(inp["conv_ln_g"]), 8), lay_pf(f(inp["conv_ln_b"]), 8)], 2)),
        "w_conv_o": f(inp["w_conv_o"]), "w_out": f(inp["w_out"]),
        "ln_vecs": np.ascontiguousarray(np.stack([lay_pf(f(inp[k]), KC) for k in ("ln1_g", "ln1_b", "ln2_g", "ln2_b")], 2)),
        "w_gr": np.ascontiguousarray(np.concatenate([f(inp["w_group"]), f(inp["w_router"])], -1)),
        "b_gr": np.ascontiguousarray(np.concatenate([f(inp["b_group"]), f(inp["b_router"])], -1)),
        "w_e_gate": f(inp["w_e_gate"][:, :1] if small else inp["w_e_gate"]), "w_e_up": f(inp["w_e_up"][:, :1] if small else inp["w_e_up"]),
        "w_e_down": f(inp["w_e_down"][:, :1] if small else inp["w_e_down"]),
        "consts": c, "cos": cos, "sin": sin,
    }
    x = f(inp["x"]); ctx = f(inp["ctx"]); cc = f(inp["c"]); c_ctx = f(inp["c_ctx"])
    maps = []
    for i in range(ncores):
        rows = np.stack([cc[2 * i], cc[2 * i + 1], c_ctx], 0)
        cT = np.ascontiguousarray(np.transpose(rows.reshape(3, KC, 128), (2, 1, 0))).reshape(128, KC * 3)
        m = dict(shared)
        m["x"] = np.ascontiguousarray(x[2 * i:2 * i + 2])
        m["ctx"] = np.ascontiguousarray(ctx[2 * i:2 * i + 2])
        m["cT"] = cT
        maps.append(m)
    return maps


def kernel(**inputs):
    nc = build()
    maps = make_in_maps(inputs)
    res = run_bass_kernel_spmd(nc, maps, core_ids=list(range(8)))
    return np.concatenate([np.asarray(r["out"], dtype=np.float32) for r in res.results], axis=0)
```
